# Optimizing a Trainium2 kernel written in Bass

```python
import jax, jax.numpy as jnp
from jax import lax
import numpy as np

D_MODEL = 4096
BATCH = 1
SEQ = 8192
DEPTH = 1

D_MIX = D_MODEL
A_WIDTH = D_MIX // 2
A_CHUNK = 128
A_GROUP_DIM = 128
A_GROUPS = A_WIDTH // A_GROUP_DIM
B_WIDTH = D_MIX - A_WIDTH
B_HEAD_DIM = 64
B_HEADS = B_WIDTH // B_HEAD_DIM
DECAY_LORA = 96
ICLR_LORA = 96
GATE_LORA = 256
B_COLS = 3 * B_WIDTH + DECAY_LORA + ICLR_LORA + GATE_LORA
IN_COLS = 2 * A_WIDTH + B_COLS
N_EXPERTS = 128
TOP_K = 8
N_GROUPS = 8
TOPK_GROUPS = 4
D_EXPERT = 384
D_SHARED = 384
ROUTED_SCALE = 2.5
MOE_BLOCK = 128
NORM_EPS = 1e-6
LN_EPS = 1e-5
GN_EPS = 64e-5
N_MOD = 6

kernel_name = "hymba_gmlp_rwkv7_moe_adaln"


def rmsnorm(x, g):
    xf = x.astype(jnp.float32)
    y = xf * lax.rsqrt(jnp.mean(xf * xf, axis=-1, keepdims=True) + NORM_EPS)
    return (y * g.astype(jnp.float32)).astype(x.dtype)


def modulate(h, shift, scale):
    return h * (1 + scale[:, None, :]) + shift[:, None, :]


def spatial_gating(p, ln_g, ln_b, spatial_w, spatial_b):
    B, T, _ = p.shape
    u, v = jnp.split(jax.nn.gelu(p, approximate=False), 2, axis=-1)
    vf = v.reshape(B, T, A_GROUPS, A_GROUP_DIM).astype(jnp.float32)
    mu = jnp.mean(vf, axis=-1, keepdims=True)
    var = jnp.mean(jnp.square(vf - mu), axis=-1, keepdims=True)
    vf = (vf - mu) * lax.rsqrt(var + LN_EPS)
    vn = (vf * ln_g.reshape(A_GROUPS, A_GROUP_DIM).astype(jnp.float32)
          + ln_b.reshape(A_GROUPS, A_GROUP_DIM).astype(jnp.float32)).astype(p.dtype)
    n_chunks = T // A_CHUNK
    vn = vn.reshape(B, n_chunks, A_CHUNK, A_GROUPS, A_GROUP_DIM)
    ws = spatial_w * jnp.tril(jnp.ones((A_CHUNK, A_CHUNK), spatial_w.dtype))
    mixed = jnp.einsum('gij,bnjgc->bnigc', ws, vn) + spatial_b.T[None, None, :, :, None]
    return u * mixed.reshape(B, T, A_WIDTH)


def rwkv7_scan(r, w, k, v, a_vec, b_vec):
    B, T, H, N = r.shape

    def step(S, inp):
        r_t, w_t, k_t, v_t, a_t, b_t = inp
        sa = jnp.einsum('bhvk,bhk->bhv', S, a_t)
        S = S * w_t[:, :, None, :] + sa[..., None] * b_t[:, :, None, :] + v_t[..., None] * k_t[:, :, None, :]
        return S, jnp.einsum('bhvk,bhk->bhv', S, r_t)

    xs = tuple(jnp.moveaxis(t, 1, 0) for t in (r, w, k, v, a_vec, b_vec))
    S0 = jnp.zeros((B, H, N, N), jnp.float32)
    _, ys = lax.scan(step, S0, xs)
    return jnp.moveaxis(ys, 0, 1)


def rwkv7_mix(p, shift_mu, decay_up, decay_base, iclr_up, iclr_base, gate_up,
              kk_scale, ka_scale, bonus, gn_g, gn_b):
    B, T, _ = p.shape
    dt = p.dtype
    prev = jnp.pad(p, ((0, 0), (1, 0), (0, 0)))[:, :-1]
    p = p + shift_mu * (prev - p)
    cuts = [B_WIDTH, 2 * B_WIDTH, 3 * B_WIDTH, 3 * B_WIDTH + DECAY_LORA, 3 * B_WIDTH + DECAY_LORA + ICLR_LORA]
    r, k, v, wd, ad, gd = jnp.split(p, cuts, axis=-1)
    w_log = -jax.nn.softplus(-(decay_base + jnp.tanh(wd) @ decay_up).astype(jnp.float32)) - 0.5
    decay = jnp.exp(-jnp.exp(w_log))
    a = jax.nn.sigmoid(iclr_base + ad @ iclr_up)
    g = jax.nn.sigmoid(gd) @ gate_up
    heads = lambda t: t.reshape(B, T, B_HEADS, B_HEAD_DIM)
    kk = heads(k * kk_scale).astype(jnp.float32)
    kk = kk / jnp.maximum(jnp.sqrt(jnp.sum(kk * kk, axis=-1, keepdims=True)), 1e-12)
    k = k * (1 + (a - 1) * ka_scale)
    rh, kh, vh = heads(r), heads(k), heads(v)
    ah = heads(a).astype(jnp.float32)
    y = rwkv7_scan(rh.astype(jnp.float32), heads(decay), kh.astype(jnp.float32),
                   vh.astype(jnp.float32), -kk, kk * ah)
    mu = jnp.mean(y, axis=-1, keepdims=True)
    var = jnp.mean(jnp.square(y - mu), axis=-1, keepdims=True)
    y = (y - mu) * lax.rsqrt(var + GN_EPS)
    y = (y * gn_g.reshape(B_HEADS, B_HEAD_DIM).astype(jnp.float32)
         + gn_b.reshape(B_HEADS, B_HEAD_DIM).astype(jnp.float32))
    y = y.astype(dt)
    y = y + jnp.sum(rh * kh * bonus, axis=-1, keepdims=True) * vh
    return y.reshape(B, T, B_WIDTH) * g


def moe_ffn(h, router_w, router_bias, exp_w_gate, exp_w_up, exp_w_down,
            sh_w_gate, sh_w_up, sh_w_down):
    B, T, D = h.shape
    n_tok = B * T
    xt = h.reshape(n_tok, D)
    scores = jax.nn.sigmoid(jnp.dot(xt, router_w).astype(jnp.float32))
    sel = scores + router_bias.astype(jnp.float32)
    grp = sel.reshape(n_tok, N_GROUPS, N_EXPERTS // N_GROUPS)
    grp_score = jnp.sum(lax.top_k(grp, 2)[0], axis=-1)
    _, top_grp = lax.top_k(grp_score, TOPK_GROUPS)
    grp_mask = jnp.any(top_grp[..., None] == jnp.arange(N_GROUPS), axis=-2)
    exp_mask = jnp.repeat(grp_mask, N_EXPERTS // N_GROUPS, axis=-1)
    _, top_e = lax.top_k(jnp.where(exp_mask, sel, -jnp.inf), TOP_K)
    gate = jnp.take_along_axis(scores, top_e, axis=-1)
    gate = gate / jnp.sum(gate, axis=-1, keepdims=True) * ROUTED_SCALE
    n_assign = n_tok * TOP_K
    n_rows = n_assign + N_EXPERTS * MOE_BLOCK
    n_blocks = n_rows // MOE_BLOCK
    e_flat = top_e.reshape(-1).astype(jnp.int32)
    tok_flat = jnp.arange(n_assign, dtype=jnp.int32) // TOP_K
    w_flat = gate.reshape(-1).astype(h.dtype)
    order = jnp.argsort(e_flat)
    e_sorted = e_flat[order]
    counts = jnp.bincount(e_flat, length=N_EXPERTS).astype(jnp.int32)
    padded = (counts + MOE_BLOCK - 1) // MOE_BLOCK * MOE_BLOCK
    start = jnp.cumsum(counts) - counts
    pstart = jnp.cumsum(padded) - padded
    dest = pstart[e_sorted] + jnp.arange(n_assign, dtype=jnp.int32) - start[e_sorted]
    row_tok = jnp.full((n_rows,), n_tok, jnp.int32).at[dest].set(tok_flat[order])
    row_w = jnp.zeros((n_rows,), h.dtype).at[dest].set(w_flat[order])
    block_e = jnp.searchsorted(jnp.cumsum(padded), jnp.arange(n_blocks, dtype=jnp.int32) * MOE_BLOCK, side='right')
    block_e = jnp.minimum(block_e, N_EXPERTS - 1).astype(jnp.int32)
    x_pad = jnp.concatenate([xt, jnp.zeros((1, D), xt.dtype)], axis=0)

    def block_body(acc, blk):
        idx, wts, e = blk
        xb = x_pad[idx]
        hb = jax.nn.silu(xb @ exp_w_gate[e]) * (xb @ exp_w_up[e])
        upd = ((hb @ exp_w_down[e]) * wts[:, None]).astype(acc.dtype)
        return acc.at[idx].add(upd), None

    acc, _ = lax.scan(block_body, jnp.zeros_like(x_pad),
                      (row_tok.reshape(n_blocks, MOE_BLOCK), row_w.reshape(n_blocks, MOE_BLOCK), block_e))
    routed = acc[:n_tok]
    shared = (jax.nn.silu(xt @ sh_w_gate) * (xt @ sh_w_up)) @ sh_w_down
    return (shared + routed).reshape(B, T, D)


def setup_inputs(seed: int = 0) -> dict:
    key = jax.random.key(seed)
    ks = jax.random.split(key, 32)
    f32 = jnp.float32
    nrm = lambda k, shape, s: jax.random.normal(k, shape, f32) * s
    D, L = D_MODEL, DEPTH
    return {
        "x": nrm(ks[0], (BATCH, SEQ, D), 1.0),
        "c": nrm(ks[1], (BATCH, D), 1.0),
        "mod_w": nrm(ks[2], (L, D, N_MOD * D), 0.5 * D ** -0.5),
        "mod_b": nrm(ks[3], (L, N_MOD * D), 0.02),
        "norm1_g": 1.0 + nrm(ks[4], (L, D), 0.02),
        "norm2_g": 1.0 + nrm(ks[5], (L, D), 0.02),
        "w_in": nrm(ks[6], (L, D, IN_COLS), D ** -0.5),
        "w_out": nrm(ks[7], (L, D_MIX, D), D_MIX ** -0.5),
        "a_ln_g": 1.0 + nrm(ks[8], (L, A_WIDTH), 0.02),
        "a_ln_b": nrm(ks[9], (L, A_WIDTH), 0.02),
        "a_spatial_w": nrm(ks[10], (L, A_GROUPS, A_CHUNK, A_CHUNK), A_CHUNK ** -0.5),
        "a_spatial_b": 1.0 + nrm(ks[11], (L, A_GROUPS, A_CHUNK), 0.02),
        "b_shift_mu": jax.random.uniform(ks[12], (L, B_COLS), f32),
        "b_decay_up": nrm(ks[13], (L, DECAY_LORA, B_WIDTH), DECAY_LORA ** -0.5),
        "b_decay_base": nrm(ks[14], (L, B_WIDTH), 1.0),
        "b_iclr_up": nrm(ks[15], (L, ICLR_LORA, B_WIDTH), ICLR_LORA ** -0.5),
        "b_iclr_base": nrm(ks[16], (L, B_WIDTH), 0.1),
        "b_gate_up": nrm(ks[17], (L, GATE_LORA, B_WIDTH), GATE_LORA ** -0.5),
        "b_kk_scale": 0.85 + nrm(ks[18], (L, B_WIDTH), 0.02),
        "b_ka_scale": 1.0 + nrm(ks[19], (L, B_WIDTH), 0.02),
        "b_bonus": nrm(ks[20], (L, B_HEADS, B_HEAD_DIM), 0.1),
        "b_gn_g": 1.0 + nrm(ks[21], (L, B_WIDTH), 0.02),
        "b_gn_b": nrm(ks[22], (L, B_WIDTH), 0.02),
        "router_w": nrm(ks[23], (L, D, N_EXPERTS), D ** -0.5),
        "router_bias": nrm(ks[24], (L, N_EXPERTS), 0.01),
        "exp_w_gate": nrm(ks[25], (L, N_EXPERTS, D, D_EXPERT), D ** -0.5),
        "exp_w_up": nrm(ks[26], (L, N_EXPERTS, D, D_EXPERT), D ** -0.5),
        "exp_w_down": nrm(ks[27], (L, N_EXPERTS, D_EXPERT, D), D_EXPERT ** -0.5),
        "sh_w_gate": nrm(ks[28], (L, D, D_SHARED), D ** -0.5),
        "sh_w_up": nrm(ks[29], (L, D, D_SHARED), D ** -0.5),
        "sh_w_down": nrm(ks[30], (L, D_SHARED, D), D_SHARED ** -0.5),
        "final_g": 1.0 + nrm(ks[31], (D,), 0.02),
    }


def reference(x, c, mod_w, mod_b, norm1_g, norm2_g, w_in, w_out,
              a_ln_g, a_ln_b, a_spatial_w, a_spatial_b,
              b_shift_mu, b_decay_up, b_decay_base, b_iclr_up, b_iclr_base, b_gate_up,
              b_kk_scale, b_ka_scale, b_bonus, b_gn_g, b_gn_b,
              router_w, router_bias, exp_w_gate, exp_w_up, exp_w_down,
              sh_w_gate, sh_w_up, sh_w_down, final_g):
    for l in range(DEPTH):
        mod = jnp.dot(jax.nn.silu(c), mod_w[l]) + mod_b[l]
        sh1, sc1, g1, sh2, sc2, g2 = jnp.split(mod, N_MOD, axis=-1)
        h = modulate(rmsnorm(x, norm1_g[l]), sh1, sc1)
        proj = jnp.einsum('btd,dc->btc', h, w_in[l])
        ya = spatial_gating(proj[..., :2 * A_WIDTH], a_ln_g[l], a_ln_b[l], a_spatial_w[l], a_spatial_b[l])
        yb = rwkv7_mix(proj[..., 2 * A_WIDTH:], b_shift_mu[l], b_decay_up[l], b_decay_base[l],
                       b_iclr_up[l], b_iclr_base[l], b_gate_up[l], b_kk_scale[l], b_ka_scale[l],
                       b_bonus[l], b_gn_g[l], b_gn_b[l])
        mix = jnp.einsum('btm,md->btd', jnp.concatenate([ya, yb], axis=-1), w_out[l])
        x = x + g1[:, None, :] * mix
        h = modulate(rmsnorm(x, norm2_g[l]), sh2, sc2)
        ffn = moe_ffn(h, router_w[l], router_bias[l], exp_w_gate[l], exp_w_up[l], exp_w_down[l],
                      sh_w_gate[l], sh_w_up[l], sh_w_down[l])
        x = x + g2[:, None, :] * ffn
    return rmsnorm(x, final_g)
```

```python
import numpy as np
from contextlib import ExitStack
import concourse.bass as bass
import concourse.mybir as mybir
from concourse.bass_utils import run_bass_kernel_spmd

F32 = mybir.dt.float32
BF16 = mybir.dt.bfloat16
I32 = mybir.dt.int32
AF = mybir.ActivationFunctionType
ALU = mybir.AluOpType
AX = mybir.AxisListType

NCORES = 2
TC = 8192 // NCORES
NHG = 8
NOB = 8 // NCORES
D = 4096
KC = D // 128
T = 8192
TOWN = 1024
NMOD = 6
A_W = 2048
B_W = 2048
HD = 64
NH = 32
DLORA = 96
GLORA = 256
B_COLS = 3 * B_W + 2 * DLORA + GLORA
IN_COLS = 2 * A_W + B_COLS
NE = 128
DE = 384
NORM_EPS = 1e-6
LN_EPS = 1e-5
GN_EPS = 64e-5
EXPM05 = float(np.exp(-0.5))

ENGS = ("pe", "act", "dve", "pool", "sp")


class Buf:
    __slots__ = ("name", "w", "r", "dsem", "dcnt")

    def __init__(self, name):
        self.name = name
        self.w = []
        self.r = []
        self.dsem = None
        self.dcnt = 0


class Sched:
    def __init__(self, nc, es):
        self.nc = nc
        self.es = es
        self.q = {e: [] for e in ENGS}
        self.cnt = {e: 0 for e in ENGS}
        self.sem = {e: es.enter_context(nc.semaphore("s_" + e)) for e in ENGS}
        self.waited = {e: {} for e in ENGS}
        self.semobj = {e: self.sem[e] for e in ENGS}
        self.dsems = {}
        self.ninst = 0

    def _dsem(self, b):
        rec = self.dsems.get(b.name)
        if rec is None:
            sem = self.es.enter_context(self.nc.semaphore("d%d_%s" % (len(self.dsems), b.name)))
            rec = {"sem": sem, "cnt": 0}
            self.dsems[b.name] = rec
            self.semobj["d:" + b.name] = sem
        return rec

    def _wait(self, eng, tok):
        kind, key, val = tok
        if kind == "eng" and key == eng and eng == "pe":
            return
        cur = self.waited[eng].get(key, 0)
        if cur >= val:
            return
        self.waited[eng][key] = val
        sem = self.semobj[key]
        self.q[eng].append(lambda e, sem=sem, val=val: e.wait_ge(sem, val))

    def _deps(self, eng, reads, writes):
        for b in reads:
            for tok in b.w:
                self._wait(eng, tok)
        for b in writes:
            for tok in b.w:
                self._wait(eng, tok)
            for tok in b.r:
                self._wait(eng, tok)

    def op(self, eng, fn, reads=(), writes=()):
        if getattr(self, "limit", None) and self.ninst >= self.limit:
            return
        self._deps(eng, reads, writes)
        self.cnt[eng] += 1
        n = self.cnt[eng]
        sem = self.sem[eng]
        self.q[eng].append(lambda e, fn=fn, sem=sem: fn(e).then_inc(sem, 1))
        tok = ("eng", eng, n)
        for b in reads:
            b.r.append(tok)
        for b in writes:
            b.w = [tok]
            b.r = []
        self.ninst += 1

    def dma(self, q, out, in_, reads=(), writes=(), owner=None, keep_w=False, **kw):
        if getattr(self, "limit", None) and self.ninst >= self.limit and not kw.pop("force", False):
            return
        kw.pop("force", None)
        if owner is None:
            owner = writes[0]
        rec = self._dsem(owner)
        sem = rec["sem"]
        okey = "d:" + owner.name
        for b in reads:
            for tok in b.w:
                self._wait(q, tok)
        for b in writes:
            if not keep_w:
                for tok in b.w:
                    self._wait(q, tok)
            for tok in b.r:
                self._wait(q, tok)
        rec["cnt"] += 16
        val = rec["cnt"]
        self.q[q].append(lambda e, out=out, in_=in_, sem=sem, kw=kw:
                         e.dma_start(out=out, in_=in_, **kw).then_inc(sem, 16))
        tok = ("dma", okey, val)
        for b in reads:
            b.r.append(tok)
        for b in writes:
            if keep_w:
                b.w = [t for t in b.w if not (t[0] == "dma" and t[1] == okey)] + [tok]
            else:
                b.w = [tok]
            b.r = []
        self.ninst += 1

    def barrier(self):
        for e in ENGS:
            for f in ENGS:
                if f != e and self.cnt[f] > 0:
                    self._wait(e, ("eng", f, self.cnt[f]))
            for name, rec in self.dsems.items():
                if rec["cnt"] > 0:
                    self._wait(e, ("dma", "d:" + name, rec["cnt"]))

    def final_wait(self, eng="sp"):
        for f in ENGS:
            if f != eng and self.cnt[f] > 0:
                self._wait(eng, ("eng", f, self.cnt[f]))
        for name, rec in self.dsems.items():
            if rec["cnt"] > 0:
                self._wait(eng, ("dma", "d:" + name, rec["cnt"]))

    def emit(self):
        nc = self.nc
        with nc.Block() as block:
            @block.tensor
            def _(e):
                for t in self.q["pe"]:
                    t(e)

            @block.scalar
            def _(e):
                for t in self.q["act"]:
                    t(e)

            @block.vector
            def _(e):
                for t in self.q["dve"]:
                    t(e)

            @block.gpsimd
            def _(e):
                for t in self.q["pool"]:
                    t(e)

            @block.sync
            def _(e):
                for t in self.q["sp"]:
                    t(e)


class Tl:
    n = 0

    def __init__(self, nc, stack, name, shape, dt, psum=False):
        f = nc.psum_tensor if psum else nc.sbuf_tensor
        Tl.n += 1
        self.t = stack.enter_context(f("t%d_%s" % (Tl.n, name), list(shape), dt))
        self.b = Buf(name)

    def __getitem__(self, k):
        return self.t[k]


def build_nc(nblk=None, stop=None, nhg=None, nob=None, limit=None, fake_mod=False):
    nc = bass.Bass("TRN2", target_bir_lowering=False)
    es = ExitStack()
    S = Sched(nc, es)
    S.limit = limit

    in_names = []
    order = ["M", "B", "A", "S", "O", "R", "E", None]

    def din(name, shape, dt=F32, need=None):
        if need is not None and order.index(stop) < order.index(need):
            return None
        in_names.append(name)
        return nc.dram_tensor(name, list(shape), dt, kind="ExternalInput").ap()

    x_in = din("x", [T, D])
    x_own = din("x_own", [TC, D], need="A")
    csel_d = din("csel", [128, NCORES], need="A")
    c_in = din("c", [KC, 128])
    mod_w = None if fake_mod else din("mod_w", [D, NMOD * D])
    mod_b = din("mod_b", [1, NMOD * D])
    n1g_d = din("n1g", [KC, 128])
    n2g_d = din("n2g", [KC, 128])
    fing_d = din("fing", [1, D])
    w_in_a = din("w_in_a", [D, 2 * A_W], need="A")
    w_in_b = din("w_in_b", [NHG, D, 1280])
    mu_d = din("mu", [NHG, 10, 128])
    w_out_d = din("w_out", [D, D], need="O")
    alng_d = din("alng", [1, A_W])
    alnb_d = din("alnb", [1, A_W])
    spw_d = din("spw", [16, 128, 128])
    spb_d = din("spb", [1, A_W])
    dup_d = din("dup", [NHG, DLORA, 256])
    iup_d = din("iup", [NHG, DLORA, 256])
    gup_d = din("gup", [NHG, GLORA, 256])
    vecs_d = din("vecs", [NHG, 16, 128])
    rw_d = din("rw", [D, NE])
    rb_d = din("rb", [1, NE])
    NEO = NE + 1
    ewg_d = din("ewg", [NEO, D, DE], need="E")
    ewu_d = din("ewu", [NEO, D, DE], need="E")
    ewd_d = din("ewd", [NEO, DE, D], need="E")
    ident_d = din("ident", [128, 128])
    bones_d = din("bones", [128, 128])
    mask4_d = din("mask4", [128, 512])
    maskl_d = din("maskl", [128, 256])
    i64s_d = din("i64s", [128, 64])
    out_d = nc.dram_tensor("out", [TC, D], F32, kind="ExternalOutput").ap()

    agin = nc.dram_tensor("agin", [24, 128], F32)
    agout = nc.dram_tensor("agout", [192, 128], F32)
    ybin = nc.dram_tensor("ybin", [256, T], BF16)
    ybout = nc.dram_tensor("ybout", [2048, T], BF16)
    x2_d = nc.dram_tensor("x2s", [TC, D], F32)
    b_agin, b_agout, b_ybin, b_ybout, b_x2 = (Buf(n) for n in ("agin", "agout", "ybin", "ybout", "x2s"))
    b_out = Buf("out")
    xin = nc.dram_tensor("xin", [TOWN, D], F32)
    xall = nc.dram_tensor("xall", [T, D], F32)
    wina_in = nc.dram_tensor("wina_in", [D // NCORES, 2 * A_W], F32)
    wina = nc.dram_tensor("wina", [D, 2 * A_W], F32)
    wout_in = nc.dram_tensor("wout_in", [D // NCORES, D], F32)
    wout = nc.dram_tensor("wout", [D, D], F32)
    x2all = nc.dram_tensor("x2all", [T, D], F32)
    part_d = nc.dram_tensor("part", [T, D], F32)
    psum_d = nc.dram_tensor("partsum", [T, D], F32)
    b_xin, b_xall, b_wina_in, b_wina, b_wout_in, b_wout, b_x2all, b_part, b_psum = (
        Buf(n) for n in ("xin", "xall", "wina_in", "wina", "wout_in", "wout", "x2all", "part", "partsum"))
    dbg = {}

    def dbg_out(name, shape, dt=F32):
        dbg[name] = nc.dram_tensor("dbg_" + name, list(shape), dt, kind="ExternalOutput").ap()
        return dbg[name]

    def finish():
        S.final_wait("sp")
        S.emit()
        nc._in_names = in_names
        nc._dbg = sorted(dbg)
        print("instr per engine", S.cnt, "dma sems", len(S.dsems))
        return nc
    ccs = es.enter_context(nc.semaphore("ccs"))
    S.semobj["ccs"] = ccs
    cc_count = [0]

    def allgather(src, dst, bsrc, bdst, kind="AllGather"):
        S._deps("pool", [bsrc], [bdst])
        cc_count[0] += 1
        n = cc_count[0]
        op = ALU.add if kind == "AllReduce" else ALU.bypass
        S.q["pool"].append(lambda e: e.collective_compute(
            kind, op, replica_groups=[list(range(NCORES))],
            ins=[src.ap().opt()], outs=[dst.ap().opt()]).then_inc(ccs, 1))
        tok = ("cc", "ccs", n)
        bsrc.r.append(tok)
        bdst.w = [tok]
        bdst.r = []

    def ACT(out, in_, func, R, W, **kw):
        S.op("act", lambda e: e.activation(out=out, in_=in_, func=func, **kw), R, W)

    def TT(eng, out, a, b, op, R, W):
        S.op(eng, lambda e: e.tensor_tensor(out=out, in0=a, in1=b, op=op), R, W)

    def TS(eng, out, a, s1, s2, op0, op1, R, W):
        if op1 is None:
            S.op(eng, lambda e: e.tensor_scalar(out=out, in0=a, scalar1=s1, scalar2=None, op0=op0), R, W)
        else:
            S.op(eng, lambda e: e.tensor_scalar(out=out, in0=a, scalar1=s1, scalar2=s2, op0=op0, op1=op1), R, W)

    def STT(out, a, s, b, op0, op1, R, W):
        S.op("dve", lambda e: e.scalar_tensor_tensor(out=out, in0=a, scalar=s, in1=b, op0=op0, op1=op1), R, W)

    def MM(out, lhsT, rhs, start, stop, R, W):
        S.op("pe", lambda e: e.matmul(out, lhsT, rhs, start=start, stop=stop), R, W)

    def CP(eng, out, in_, R, W):
        if eng == "act":
            S.op("act", lambda e: e.copy(out=out, in_=in_), R, W)
        else:
            S.op(eng, lambda e: e.tensor_copy(out=out, in_=in_), R, W)

    def RECIP(out, in_, R, W):
        S.op("dve", lambda e: e.reciprocal(out=out, in_=in_), R, W)

    def MEMSET(eng, out, val, W):
        S.op(eng, lambda e: e.memset(out, val), [], W)

    P = ExitStack()
    es.enter_context(P)

    def tile(name, shape, dt=F32, stack=None):
        return Tl(nc, stack or P, name, shape, dt)

    PS = [Tl(nc, P, "ps%d" % i, [128, 512], F32, psum=True) for i in range(8)]
    psi = [0]

    def ps():
        psi[0] = (psi[0] + 1) % 8
        return PS[psi[0]]

    ident = tile("ident", [128, 128])
    bones = tile("bones", [128, 128])
    mask4 = tile("mask4", [128, 512])
    maskl = tile("maskl", [128, 256])
    i64s = tile("i64s", [128, 64])
    i2 = tile("i2", [128, 256])
    ones = tile("ones", [128, 128])
    S.dma("sp", ident[:], ident_d, writes=[ident.b])
    S.dma("sp", bones[:], bones_d, writes=[bones.b])
    S.dma("sp", mask4[:], mask4_d, writes=[mask4.b])
    S.dma("sp", maskl[:], maskl_d, writes=[maskl.b])
    S.dma("sp", i64s[:], i64s_d, writes=[i64s.b])
    S.dma("sp", i2[:, 0:128], ident_d, writes=[i2.b])
    S.dma("sp", i2[:, 128:256], ident_d, writes=[i2.b], keep_w=True)
    MEMSET("pool", ones[:], 1.0, [ones.b])

    def TR(out, in_, k, R, W):
        S.op("pe", lambda e: e.transpose(out, in_, ident[0:k, 0:k]), list(R) + [ident.b], W)

    def load_pl(name, dram_rows, k, stack=None):
        o = tile(name, [128, k], F32, stack)
        with ExitStack() as tmp:
            rows = Tl(nc, tmp, name + "_rows", [k, 128], F32)
            S.dma("sp", rows[:], dram_rows, writes=[rows.b])
            p = ps()
            TR(p[:, 0:k], rows[:], k, [rows.b], [p.b])
            CP("dve", o[:], p[:, 0:k], [p.b], [o.b])
            S.barrier()
        return o

    cT = load_pl("cT", c_in, KC)
    ACT(cT[:], cT[:], AF.Silu, [cT.b], [cT.b])
    n1g = load_pl("n1gT", n1g_d, KC)
    n2g = load_pl("n2gT", n2g_d, KC)
    modT = tile("modT", [128, 192])
    with ExitStack() as ph:
        mw = [Tl(nc, ph, "mw%d" % i, [128, 3072], F32) for i in range(3)]
        mrow = Tl(nc, ph, "mrow", [1, 3072], F32)
        mbrow = Tl(nc, ph, "mbrow", [1, 3072], F32)
        mrows = [Tl(nc, ph, "mrows%d" % i, [96, 128], F32) for i in range(2)]
        if fake_mod:
            MEMSET("pool", mrow[:], 0.1, [mrow.b])
            for gq in range(8):
                S.dma("sp", agout.ap()[gq * 24:(gq + 1) * 24, :].rearrange("(o a) b -> o (a b)", o=1), mrow[:],
                      reads=[mrow.b], writes=[b_agout], keep_w=True)
        for gq in range(0 if fake_mod else 8):
            gsl = slice(gq * 3072, (gq + 1) * 3072)
            S.dma("sp", mbrow[:], mod_b[:, gsl], writes=[mbrow.b])
            for k in range(KC):
                w = mw[k % 3]
                S.dma("sp", w[:], mod_w[k * 128:(k + 1) * 128, gsl], writes=[w.b])
                for b in range(6):
                    MM(PS[b][0:1, :], cT[:, k:k + 1], w[:, b * 512:(b + 1) * 512], k == 0, k == KC - 1,
                       [cT.b, w.b], [PS[b].b])
            for b in range(6):
                TT("dve", mrow[:, b * 512:(b + 1) * 512], PS[b][0:1, :], mbrow[:, b * 512:(b + 1) * 512], ALU.add,
                   [PS[b].b, mbrow.b], [mrow.b])
            S.dma("sp", agout.ap()[gq * 24:(gq + 1) * 24, :].rearrange("(o a) b -> o (a b)", o=1), mrow[:],
                  reads=[mrow.b], writes=[b_agout], keep_w=True)
        for i in range(2):
            S.dma("sp", mrows[i][:], agout.ap()[i * 96:(i + 1) * 96, :], reads=[b_agout], writes=[mrows[i].b])
            p = ps()
            TR(p[:, 0:96], mrows[i][:], 96, [mrows[i].b], [p.b])
            CP("dve", modT[:, i * 96:(i + 1) * 96], p[:, 0:96], [p.b], [modT.b])
        S.barrier()
    s1 = tile("s1", [128, KC])
    s2 = tile("s2", [128, KC])
    STT(s1[:], modT[:, 32:64], 1.0, n1g[:], ALU.add, ALU.mult, [modT.b, n1g.b], [s1.b])
    STT(s2[:], modT[:, 128:160], 1.0, n2g[:], ALU.add, ALU.mult, [modT.b, n2g.b], [s2.b])
    sh1 = modT[:, 0:32]
    sh2 = modT[:, 96:128]
    if stop == "M":
        S.dma("sp", dbg_out("modT", [128, 192]), modT[:], reads=[modT.b], writes=[Buf("dbgm")])
        return finish()

    def bc_row(dst, src_row_ap, R, W):
        S.dma("sp", dst, src_row_ap.partition_broadcast(128).squeeze(1), reads=R, writes=W)

    junk = tile("junk", [128, D], BF16)
    ssq = tile("ssq", [128, 1])
    rstd = tile("rstd", [128, 1])

    def norm_T(xt, sc, sh, shb, dsts):
        ACT(junk[:], xt[:], AF.Square, [xt.b], [junk.b, ssq.b], accum_out=ssq[:])
        TS("dve", rstd[:], ssq[:], 1.0 / D, NORM_EPS, ALU.mult, ALU.add, [ssq.b], [rstd.b])
        ACT(rstd[:], rstd[:], AF.Sqrt, [rstd.b], [rstd.b])
        RECIP(rstd[:], rstd[:], [rstd.b], [rstd.b])
        TS("dve", xt[:], xt[:], rstd[:, 0:1], None, ALU.mult, None, [xt.b, rstd.b], [xt.b])
        for c4 in range(KC // 4):
            p = ps()
            for j in range(4):
                c = c4 * 4 + j
                TR(p[:, j * 128:(j + 1) * 128], xt[:, c * 128:(c + 1) * 128], 128, [xt.b], [p.b])
            for j in range(4):
                c = c4 * 4 + j
                for fn, b in dsts:
                    ACT(fn(c), p[:, j * 128:(j + 1) * 128], AF.Identity, [p.b, sc.b, shb], [b],
                        scale=sc[:, c:c + 1], bias=sh[:, c:c + 1])

    def load_w_bf16(dst_fn, src_rows_fn, nchunks, ncols, stg, R_extra=()):
        for c in range(nchunks):
            st = stg[c % len(stg)]
            S.dma("sp", st[:, 0:ncols], src_rows_fn(c), reads=list(R_extra), writes=[st.b])
            ap, b = dst_fn(c)
            CP("pool", ap, st[:, 0:ncols], [st.b], [b])

    NHG_RUN = nhg or NHG
    for hg in range(NHG_RUN):
        C0 = EXPM05
        BLK = 256
        NBLK = nblk or (T // BLK)
        with ExitStack() as ph:
            def pt(name, shape, dt=F32):
                return Tl(nc, ph, name, shape, dt)
            vecs = load_pl("vecsT", vecs_d[hg], 16, ph)
            muT = load_pl("muT", mu_d[hg], 10, ph)
            omka = pt("omka", [128, 2])
            TS("dve", omka[:], vecs[:, 6:8], -1.0, 1.0, ALU.mult, ALU.add, [vecs.b], [omka.b])
            WB = pt("WB", [128, KC, 1280], BF16)
            dupw = pt("dupw", [DLORA, 256], BF16)
            iupw = pt("iupw", [DLORA, 256], BF16)
            gupw = pt("gupw", [128, 2, 256], BF16)
            with ExitStack() as tmp:
                stg = [Tl(nc, tmp, "stgB%d" % i, [128, 1280], F32) for i in range(2)]
                load_w_bf16(lambda c: (WB[:, c, :], WB.b), lambda c: w_in_b[hg, c * 128:(c + 1) * 128, :], KC, 1280, stg)
                S.dma("sp", stg[0][0:DLORA, 0:256], dup_d[hg], writes=[stg[0].b])
                CP("pool", dupw[:], stg[0][0:DLORA, 0:256], [stg[0].b], [dupw.b])
                S.dma("sp", stg[1][0:DLORA, 0:256], iup_d[hg], writes=[stg[1].b])
                CP("pool", iupw[:], stg[1][0:DLORA, 0:256], [stg[1].b], [iupw.b])
                for j in range(2):
                    S.dma("sp", stg[j][:, 0:256], gup_d[hg, j * 128:(j + 1) * 128, :], writes=[stg[j].b])
                    CP("pool", gupw[:, j, :], stg[j][:, 0:256], [stg[j].b], [gupw.b])
                S.barrier()
            xts = [pt("xtB0", [128, D])] * 2
            h1T = pt("h1T", [128, KC, BLK], BF16)
            pB = [pt("pB%d" % i, [128, BLK + 1]) for i in range(10)]
            pS = [pt("pS%d" % i, [128, BLK]) for i in range(10)]
            tmpd = pt("tmpd", [128, BLK])
            twd = pt("twd", [128, BLK], BF16)
            adb = pt("adb", [128, BLK], BF16)
            sgd = pt("sgd", [128, 2, BLK], BF16)
            sg = pt("sg", [128, BLK]); av = pt("av", [128, BLK]); gate = [pt("gate%d" % i, [128, BLK]) for i in range(2)]
            sq = pt("sq", [128, BLK]); rinv = pt("rinv", [128, BLK]); kk = pt("kk", [128, BLK])
            tk = pt("tk", [128, BLK]); kmod = pt("kmod", [128, BLK]); bvec = pt("bvec", [128, BLK])
            rkb = pt("rkb", [128, BLK]); bterm = [pt("bterm%d" % i, [128, BLK]) for i in range(2)]
            Gc = pt("Gc", [128, BLK]); Gx = pt("Gx", [128, BLK]); nb = pt("nb", [128, 2]); PCt = pt("PCt", [128, 2])
            E1 = pt("E1", [128, BLK]); Em = pt("Em", [128, BLK]); Ep = pt("Ep", [128, BLK]); Ee = pt("Ee", [128, BLK])
            BKt = [pt("BKt%d" % i, [128, 2, BLK], BF16) for i in range(2)]
            ARt = [pt("ARt%d" % i, [128, 2, BLK], BF16) for i in range(2)]
            TM = [pt("TM%d" % i, [128, 4, BLK]) for i in range(2)]
            TOK = pt("TOK", [128, 4, 128], BF16)
            ABm = pt("ABm", [128, 512], BF16); AKm = pt("AKm", [128, 512], BF16)
            Lk = [pt("Lk%d" % i, [128, 256], BF16) for i in range(2)]
            Mk = [pt("Mk%d" % i, [128, 256], BF16) for i in range(2)]
            Tt = [pt("Tt%d" % i, [128, 256], BF16) for i in range(2)]
            Xs = pt("Xs", [128, 128], BF16)
            WU = pt("WU", [128, 256], BF16)
            QT = pt("QT", [128, 128], BF16)
            Mc = pt("Mc", [128, 64], BF16)
            NcT = pt("NcT", [128, 64])
            ST = [[pt("ST%d_%d" % (ct, i), [128, 64], BF16) for i in range(2)] for ct in range(2)]
            st6 = pt("st6", [128, 2, 6]); mv = pt("mv", [128, 2, 2]); grs = pt("grs", [128, 2])
            yn = pt("yn", [128, 128]); ybn = pt("ybn", [128, 128])
            ybo = [pt("ybo%d" % i, [128, BLK], BF16) for i in range(2)]
            for i in range(10):
                MEMSET("pool", pB[i][:, 0:1], 0.0, [pB[i].b])
            for ct in range(2):
                MEMSET("pool", ST[ct][0][:], 0.0, [ST[ct][0].b])
            stp = [0, 0]

            for blk in range(NBLK):
                for tt in range(BLK // 128):
                    xt = xts[tt % 2]
                    r0 = blk * BLK + tt * 128
                    S.dma("sp", xt[:], x_in[r0:r0 + 128, :], writes=[xt.b])
                    norm_T(xt, s1, sh1, modT.b,
                           [(lambda c, tt=tt: h1T[:, c, tt * 128:(tt + 1) * 128], h1T.b)])
                for ci in range(10):
                    p = ps()
                    for k in range(KC):
                        MM(p[:, 0:BLK], WB[:, k, ci * 128:(ci + 1) * 128], h1T[:, k, :], k == 0, k == KC - 1,
                           [WB.b, h1T.b], [p.b])
                    CP("act", pB[ci][:, 1:BLK + 1], p[:, 0:BLK], [p.b], [pB[ci].b])
                    TT("dve", tmpd[:], pB[ci][:, 0:BLK], pB[ci][:, 1:BLK + 1], ALU.subtract, [pB[ci].b], [tmpd.b])
                    STT(pS[ci][:], tmpd[:], muT[:, ci:ci + 1], pB[ci][:, 1:BLK + 1], ALU.mult, ALU.add,
                        [tmpd.b, muT.b, pB[ci].b], [pS[ci].b])
                    CP("pool", pB[ci][:, 0:1], pB[ci][:, BLK:BLK + 1], [pB[ci].b], [pB[ci].b])
                ACT(twd[0:DLORA, :], pS[6][0:DLORA, :], AF.Tanh, [pS[6].b], [twd.b])
                CP("pool", adb[0:DLORA, :], pS[7][0:DLORA, :], [pS[7].b], [adb.b])
                for j in range(2):
                    ACT(sgd[:, j, :], pS[8 + j][:], AF.Sigmoid, [pS[8 + j].b], [sgd.b])
                for ct in range(2):
                    cs = slice(ct * 128, (ct + 1) * 128)
                    r_, k_, v_ = pS[ct], pS[2 + ct], pS[4 + ct]
                    p = ps()
                    MM(p[:, 0:BLK], dupw[:, cs], twd[0:DLORA, :], True, True, [dupw.b, twd.b], [p.b])
                    ACT(sg[:], p[:, 0:BLK], AF.Sigmoid, [p.b, vecs.b], [sg.b], bias=vecs[:, 0 + ct:1 + ct])
                    p = ps()
                    MM(p[:, 0:BLK], iupw[:, cs], adb[0:DLORA, :], True, True, [iupw.b, adb.b], [p.b])
                    ACT(av[:], p[:, 0:BLK], AF.Sigmoid, [p.b, vecs.b], [av.b], bias=vecs[:, 2 + ct:3 + ct])
                    p = ps()
                    for j in range(2):
                        MM(p[:, 0:BLK], gupw[:, j, cs], sgd[:, j, :], j == 0, j == 1, [gupw.b, sgd.b], [p.b])
                    CP("act", gate[ct][:], p[:, 0:BLK], [p.b], [gate[ct].b])
                    ACT(sq[:], k_[:], AF.Square, [k_.b, vecs.b], [sq.b], scale=vecs[:, 4 + ct:5 + ct])
                    p = ps()
                    MM(p[:, 0:BLK], bones[:], sq[:], True, True, [bones.b, sq.b], [p.b])
                    ACT(rinv[:], p[:, 0:BLK], AF.Sqrt, [p.b], [rinv.b])
                    TS("dve", rinv[:], rinv[:], 1e-12, None, ALU.max, None, [rinv.b], [rinv.b])
                    RECIP(rinv[:], rinv[:], [rinv.b], [rinv.b])
                    STT(kk[:], k_[:], vecs[:, 4 + ct:5 + ct], rinv[:], ALU.mult, ALU.mult, [k_.b, vecs.b, rinv.b], [kk.b])
                    TS("dve", tk[:], av[:], vecs[:, 6 + ct:7 + ct], omka[:, ct:ct + 1], ALU.mult, ALU.add,
                       [av.b, vecs.b, omka.b], [tk.b])
                    TT("pool", kmod[:], k_[:], tk[:], ALU.mult, [k_.b, tk.b], [kmod.b])
                    TT("pool", bvec[:], kk[:], av[:], ALU.mult, [kk.b, av.b], [bvec.b])
                    STT(rkb[:], r_[:], vecs[:, 8 + ct:9 + ct], kmod[:], ALU.mult, ALU.mult, [r_.b, vecs.b, kmod.b], [rkb.b])
                    p = ps()
                    MM(p[:, 0:BLK], bones[:], rkb[:], True, True, [bones.b, rkb.b], [p.b])
                    TT("dve", bterm[ct][:], p[:, 0:BLK], v_[:], ALU.mult, [p.b, v_.b], [bterm[ct].b])
                    for u in range(2):
                        us = slice(u * 128, (u + 1) * 128)
                        S.op("dve", lambda e, us=us: e.tensor_tensor_scan(out=Gc[:, us], data0=ones[:, 0:128], data1=sg[:, us],
                                                                        initial=0.0, op0=ALU.mult, op1=ALU.add),
                             [ones.b, sg.b], [Gc.b])
                        TS("dve", nb[:, u:u + 1], Gc[:, u * 128 + 127:u * 128 + 128], -C0, None, ALU.mult, None, [Gc.b], [nb.b])
                    TT("pool", Gx[:], Gc[:], sg[:], ALU.subtract, [Gc.b, sg.b], [Gx.b])
                    ACT(E1[:], Gc[:], AF.Exp, [Gc.b], [E1.b], scale=-C0)
                    ACT(Em[:], Gc[:], AF.Exp, [Gc.b], [Em.b], scale=C0)
                    ACT(Ep[:], Gx[:], AF.Exp, [Gx.b], [Ep.b], scale=-C0)
                    for u in range(2):
                        us = slice(u * 128, (u + 1) * 128)
                        ACT(Ee[:, us], Gc[:, us], AF.Exp, [Gc.b, nb.b], [Ee.b], scale=C0, bias=nb[:, u:u + 1])
                    ACT(PCt[:], nb[:], AF.Exp, [nb.b], [PCt.b])
                    bk, ar, tm = BKt[ct], ARt[ct], TM[ct]
                    TT("dve", bk[:, 0, :], bvec[:], Em[:], ALU.mult, [bvec.b, Em.b], [bk.b])
                    TT("dve", bk[:, 1, :], kmod[:], Em[:], ALU.mult, [kmod.b, Em.b], [bk.b])
                    STT(tm[:, 0, :], kk[:], -1.0, Ep[:], ALU.mult, ALU.mult, [kk.b, Ep.b], [tm.b])
                    CP("pool", ar[:, 0, :], tm[:, 0, :], [tm.b], [ar.b])
                    TT("dve", ar[:, 1, :], r_[:], E1[:], ALU.mult, [r_.b, E1.b], [ar.b])
                    CP("pool", tm[:, 1, :], v_[:], [v_.b], [tm.b])
                    TT("pool", tm[:, 2, :], bvec[:], Ee[:], ALU.mult, [bvec.b, Ee.b], [tm.b])
                    TT("pool", tm[:, 3, :], kmod[:], Ee[:], ALU.mult, [kmod.b, Ee.b], [tm.b])

                    for u in range(2):
                        us = slice(u * 128, (u + 1) * 128)
                        p = ps()
                        for j in range(4):
                            TR(p[:, j * 128:(j + 1) * 128], tm[:, j, us], 128, [tm.b], [p.b])
                        CP("act", TOK[:].rearrange("p a b -> p (a b)"), p[:, :], [p.b], [TOK.b])
                        for h in range(2):
                            hp = slice(h * 64, (h + 1) * 64)
                            pa = ps()
                            MM(pa[:, 0:256], bk[hp, 0, us], ar[hp, :, us], True, True, [bk.b, ar.b], [pa.b])
                            MM(pa[:, 256:512], bk[hp, 1, us], ar[hp, :, us], True, True, [bk.b, ar.b], [pa.b])
                            pc = ps()
                            MM(pc[:, 0:128], ar[hp, 0, us], bk[hp, 0, us], True, True, [bk.b, ar.b], [pc.b])
                            TT("dve", ABm[:, h * 256:(h + 1) * 256], pa[:, 0:256], mask4[:, 0:256], ALU.mult, [pa.b, mask4.b], [ABm.b])
                            TT("dve", AKm[:, h * 256:(h + 1) * 256], pa[:, 256:512], mask4[:, 0:256], ALU.mult, [pa.b, mask4.b], [AKm.b])
                            TT("dve", Lk[0][:, h * 128:(h + 1) * 128], pc[:, 0:128], maskl[:, 0:128], ALU.mult, [pc.b, maskl.b], [Lk[0].b])
                        AB3 = ABm[:].rearrange("p (h x) -> p h x", h=2)
                        AK3 = AKm[:].rearrange("p (h x) -> p h x", h=2)
                        CP("pool", Mk[0][:].rearrange("p (h x) -> p h x", h=2), AB3[:, :, 0:128], [ABm.b], [Mk[0].b])
                        TT("pool", Tt[0][:], Mk[0][:], i2[:], ALU.add, [Mk[0].b, i2.b], [Tt[0].b])
                        px = ps()
                        for h in range(2):
                            MM(px[:, h * 64:(h + 1) * 64], AK3[:, h, 0:128], TOK[:, 1, h * 64:(h + 1) * 64], True, True,
                               [AKm.b, TOK.b], [px.b])
                        CP("act", Xs[:], px[:, 0:128], [px.b], [Xs.b])
                        ci_, ti_ = 0, 0
                        for it in range(6):
                            mk, lk, tcur = Mk[ci_], Lk[ci_], Tt[ti_]
                            mk2, lk2, tnew = Mk[1 - ci_], Lk[1 - ci_], Tt[1 - ti_]
                            if it < 5:
                                pm = ps()
                                for h in range(2):
                                    hs = slice(h * 128, (h + 1) * 128)
                                    MM(pm[:, hs], lk[:, hs], mk[:, hs], True, True, [lk.b, mk.b], [pm.b])
                            pl = ps()
                            for h in range(2):
                                hs = slice(h * 128, (h + 1) * 128)
                                MM(pl[:, hs], mk[:, hs], lk[:, hs], True, True, [lk.b, mk.b], [pl.b])
                            if it < 5:
                                CP("act", mk2[:], pm[:, 0:256], [pm.b], [mk2.b])
                            CP("dve", lk2[:], pl[:, 0:256], [pl.b], [lk2.b])
                            pt_ = ps()
                            for h in range(2):
                                hs = slice(h * 128, (h + 1) * 128)
                                MM(pt_[:, hs], lk2[:, hs], tcur[:, hs], True, True, [lk2.b, tcur.b], [pt_.b])
                            TT("dve", tnew[:], pt_[:, 0:256], tcur[:], ALU.add, [pt_.b, tcur.b], [tnew.b])
                            ci_, ti_ = 1 - ci_, 1 - ti_
                        tfin = Tt[ti_]
                        pw = ps()
                        for h in range(2):
                            hs = slice(h * 128, (h + 1) * 128)
                            MM(pw[:, h * 128:h * 128 + 64], tfin[:, hs], TOK[:, 0, h * 64:(h + 1) * 64], True, True,
                               [tfin.b, TOK.b], [pw.b])
                            MM(pw[:, h * 128 + 64:h * 128 + 128], tfin[:, hs], Xs[:, h * 64:(h + 1) * 64], True, True,
                               [tfin.b, Xs.b], [pw.b])
                        CP("act", WU[:], pw[:, 0:256], [pw.b], [WU.b])
                        pq = ps()
                        for h in range(2):
                            hp = slice(h * 64, (h + 1) * 64)
                            MM(pq[hp, 0:128], WU[:, h * 128:h * 128 + 64], AB3[:, h, 128:256], True, True, [WU.b, ABm.b], [pq.b])
                            MM(pq[hp, 128:192], WU[:, h * 128:h * 128 + 64], TOK[:, 2, h * 64:(h + 1) * 64], True, True,
                               [WU.b, TOK.b], [pq.b])
                        TT("dve", QT[:], pq[:, 0:128], ar[:, 1, us], ALU.add, [pq.b, ar.b], [QT.b])
                        STT(Mc[:], i64s[:], PCt[:, u:u + 1], pq[:, 128:192], ALU.mult, ALU.add, [i64s.b, PCt.b, pq.b], [Mc.b])
                        pn = ps()
                        for h in range(2):
                            hp = slice(h * 64, (h + 1) * 64)
                            MM(pn[hp, 0:64], TOK[:, 2, h * 64:(h + 1) * 64], WU[:, h * 128 + 64:h * 128 + 128], True, False,
                               [TOK.b, WU.b], [pn.b])
                            MM(pn[hp, 0:64], TOK[:, 3, h * 64:(h + 1) * 64], TOK[:, 1, h * 64:(h + 1) * 64], False, True,
                               [TOK.b], [pn.b])
                        CP("act", NcT[:], pn[:, 0:64], [pn.b], [NcT.b])
                        sold, snew = ST[ct][stp[ct]], ST[ct][1 - stp[ct]]
                        pys = [ps(), ps()]
                        for h in range(2):
                            hp = slice(h * 64, (h + 1) * 64)
                            py = pys[h]
                            yo = py[:, 0:64]
                            MM(yo, AB3[:, h, 128:256], WU[:, h * 128 + 64:h * 128 + 128], True, False, [ABm.b, WU.b], [py.b])
                            MM(yo, AK3[:, h, 128:256], TOK[:, 1, h * 64:(h + 1) * 64], False, False, [AKm.b, TOK.b], [py.b])
                            MM(yo, QT[hp, :], sold[hp, :], False, True, [QT.b, sold.b], [py.b])
                        for h in range(2):
                            hp = slice(h * 64, (h + 1) * 64)
                            pst = ps()
                            MM(pst[hp, 0:64], Mc[hp, :], sold[hp, :], True, True, [Mc.b, sold.b], [pst.b])
                            TT("dve", snew[hp, :], pst[hp, 0:64], NcT[hp, :], ALU.add, [pst.b, NcT.b, snew.b], [snew.b])
                        stp[ct] = 1 - stp[ct]
                        for h in range(2):
                            S.op("dve", lambda e, h=h, py=pys[h]: e.bn_stats(out=st6[:, h, :], in_=py[:, 0:64]), [pys[h].b], [st6.b])
                            S.op("dve", lambda e, h=h: e.bn_aggr(out=mv[:, h, :], in_=st6[:, h, :]), [st6.b], [mv.b])
                        TS("dve", grs[:], mv[:, :, 1], GN_EPS, None, ALU.add, None, [mv.b], [grs.b])
                        ACT(grs[:], grs[:], AF.Sqrt, [grs.b], [grs.b])
                        RECIP(grs[:], grs[:], [grs.b], [grs.b])
                        for h in range(2):
                            TS("dve", yn[:, h * 64:(h + 1) * 64], pys[h][:, 0:64], mv[:, h, 0:1], grs[:, h:h + 1],
                               ALU.subtract, ALU.mult, [pys[h].b, mv.b, grs.b, yn.b], [yn.b])
                        pz = ps()
                        TR(pz[:, 0:128], yn[:], 128, [yn.b], [pz.b])
                        ACT(ybn[:], pz[:, 0:128], AF.Identity, [pz.b, vecs.b], [ybn.b],
                            scale=vecs[:, 10 + ct:11 + ct], bias=vecs[:, 12 + ct:13 + ct])
                        TT("pool", ybn[:], ybn[:], bterm[ct][:, us], ALU.add, [ybn.b, bterm[ct].b], [ybn.b])
                        TT("pool", ybo[ct][:, us], ybn[:], gate[ct][:, us], ALU.mult, [ybn.b, gate[ct].b], [ybo[ct].b])
                    S.dma("act", ybout.ap()[hg * 256 + ct * 128:hg * 256 + (ct + 1) * 128, blk * BLK:(blk + 1) * BLK], ybo[ct][:],
                          reads=[ybo[ct].b], writes=[b_ybout], keep_w=True)
            S.barrier()

    if stop == "B":
        dy = dbg_out("yb", [NHG_RUN * 256, (nblk or 32) * 256], BF16)
        S.dma("sp", dy, ybout.ap()[0:NHG_RUN * 256, 0:(nblk or 32) * 256], reads=[b_ybout], writes=[Buf("dbgy")], force=True)
        return finish()

    NOB_RUN = nob or NOB
    for ob in range(NOB_RUN):
        tok0 = ob * TOWN
        MID = ExitStack()
        yaT = tile("yaT", [128, 16, TOWN], BF16, MID)
        HALF = 512
        with ExitStack() as ph:
            def pt(name, shape, dt=F32):
                return Tl(nc, ph, name, shape, dt)
            h1o = pt("h1o", [128, KC, HALF], BF16)
            vn = pt("vn", [128, 4, A_W], BF16)
            wsT = pt("wsT", [128, 16, 128], BF16)
            lng = pt("lng", [128, A_W]); lnb = pt("lnb", [128, A_W]); spb = pt("spb", [128, A_W])
            bc_row(lng[:], alng_d, [], [lng.b]); bc_row(lnb[:], alnb_d, [], [lnb.b]); bc_row(spb[:], spb_d, [], [spb.b])
            Wv = pt("Wv", [128, KC, 512], BF16)
            Wu = pt("Wu", [128, KC, 128], BF16)
            stg = [pt("stgA%d" % i, [128, 512]) for i in range(2)]
            vf = pt("vf", [128, 512]); vt = pt("vt", [128, 512])
            ast6 = pt("ast6", [128, 4, 6]); amv = pt("amv", [128, 4, 2]); ars = pt("ars", [128, 4])
            uT = pt("uT", [128, HALF]); mtmp = pt("mtmp", [128, 128])
            xts = [pt("xtA0", [128, D])] * 2
            for g in range(16):
                S.dma("sp", stg[g % 2][:, 0:128], spw_d[g], writes=[stg[g % 2].b])
                p = ps()
                TR(p[:, 0:128], stg[g % 2][:, 0:128], 128, [stg[g % 2].b], [p.b])
                TT("dve", wsT[:, g, :], p[:, 0:128], mask4[:, 128:256], ALU.mult, [p.b, mask4.b], [wsT.b])
            for hf in range(2):
                for tt in range(4):
                    xt = xts[tt % 2]
                    r0 = hf * HALF + tt * 128
                    S.dma("sp", xt[:], x_own[tok0 + r0:tok0 + r0 + 128, :], writes=[xt.b])
                    norm_T(xt, s1, sh1, modT.b, [(lambda c, tt=tt: h1o[:, c, tt * 128:(tt + 1) * 128], h1o.b)])
                for cb in range(4):
                    load_w_bf16(lambda c: (Wv[:, c, :], Wv.b),
                                lambda c, cb=cb: w_in_a[c * 128:(c + 1) * 128, A_W + cb * 512:A_W + (cb + 1) * 512], KC, 512, stg)
                    for tt in range(4):
                        p = ps()
                        for k in range(KC):
                            MM(p[:, :], h1o[:, k, tt * 128:(tt + 1) * 128], Wv[:, k, :], k == 0, k == KC - 1, [h1o.b, Wv.b], [p.b])
                        ACT(vf[:], p[:, :], AF.Gelu, [p.b], [vf.b])
                        for g in range(4):
                            gs = slice(g * 128, (g + 1) * 128)
                            S.op("dve", lambda e, g=g, gs=gs: e.bn_stats(out=ast6[:, g, :], in_=vf[:, gs]), [vf.b], [ast6.b])
                            S.op("dve", lambda e, g=g: e.bn_aggr(out=amv[:, g, :], in_=ast6[:, g, :]), [ast6.b], [amv.b])
                        TS("dve", ars[:], amv[:, :, 1], LN_EPS, None, ALU.add, None, [amv.b], [ars.b])
                        ACT(ars[:], ars[:], AF.Sqrt, [ars.b], [ars.b])
                        RECIP(ars[:], ars[:], [ars.b], [ars.b])
                        for g in range(4):
                            gs = slice(g * 128, (g + 1) * 128)
                            TS("dve", vt[:, gs], vf[:, gs], amv[:, g, 0:1], ars[:, g:g + 1], ALU.subtract, ALU.mult,
                               [vf.b, amv.b, ars.b], [vt.b])
                        cs = slice(cb * 512, (cb + 1) * 512)
                        TT("pool", vt[:], vt[:], lng[:, cs], ALU.mult, [vt.b, lng.b], [vt.b])
                        TT("pool", vn[:, tt, cs], vt[:], lnb[:, cs], ALU.add, [vt.b, lnb.b], [vn.b])
                for g in range(16):
                    load_w_bf16(lambda c: (Wu[:, c, :], Wu.b),
                                lambda c, g=g: w_in_a[c * 128:(c + 1) * 128, g * 128:(g + 1) * 128], KC, 128, stg)
                    p = ps()
                    for k in range(KC):
                        MM(p[:, :], Wu[:, k, :], h1o[:, k, :], k == 0, k == KC - 1, [Wu.b, h1o.b], [p.b])
                    ACT(uT[:], p[:, :], AF.Gelu, [p.b], [uT.b])
                    for tt in range(4):
                        p = ps()
                        MM(p[:, 0:128], vn[:, tt, g * 128:(g + 1) * 128], wsT[:, g, :], True, True, [vn.b, wsT.b], [p.b])
                        TT("dve", mtmp[:], p[:, 0:128], spb[:, g * 128:(g + 1) * 128], ALU.add, [p.b, spb.b], [mtmp.b])
                        c0 = hf * HALF + tt * 128
                        TT("pool", yaT[:, g, c0:c0 + 128], mtmp[:], uT[:, tt * 128:(tt + 1) * 128], ALU.mult,
                           [mtmp.b, uT.b], [yaT.b])
            S.barrier()

        ybT = tile("ybT", [128, 16, TOWN], BF16, MID)
        csel = tile("csel", [128, NCORES], F32, MID)
        S.dma("sp", csel[:], csel_d, writes=[csel.b])
        with ExitStack() as ysl:
            ycand = [Tl(nc, ysl, "ycand%d" % i, [128, TOWN], BF16) for i in range(2)]
            for ctg in range(16):
                for r in range(NCORES):
                    yc = ycand[r % 2]
                    S.dma("sp", yc[:], ybout.ap()[ctg * 128:(ctg + 1) * 128, r * TC + tok0:r * TC + tok0 + TOWN],
                          reads=[b_ybout], writes=[yc.b])
                    if r == 0:
                        TS("dve", ybT[:, ctg, :], yc[:], csel[:, 0:1], None, ALU.mult, None, [yc.b, csel.b], [ybT.b])
                    else:
                        STT(ybT[:, ctg, :], yc[:], csel[:, r:r + 1], ybT[:, ctg, :], ALU.mult, ALU.add,
                            [yc.b, csel.b, ybT.b], [ybT.b])
            S.barrier()

        with ExitStack() as ph:
            def pt(name, shape, dt=F32):
                return Tl(nc, ph, name, shape, dt)
            Wo = pt("Wo", [128, KC, 512], BF16)
            stg = [pt("stgO%d" % i, [128, 512]) for i in range(2)]
            g1bc = pt("g1bc", [128, D])
            xs_ = [pt("xsl%d" % i, [128, 512]) for i in range(2)]
            x2t = [pt("x2t%d" % i, [128, 512]) for i in range(2)]
            bc_row(g1bc[:], agout.ap()[64:96, :].rearrange("(o a) b -> o (a b)", o=1), [b_agout], [g1bc.b])
            for db in range(8):
                ds_ = slice(db * 512, (db + 1) * 512)
                load_w_bf16(lambda c: (Wo[:, c, :], Wo.b), lambda c, ds_=ds_: w_out_d[c * 128:(c + 1) * 128, ds_], KC, 512, stg)
                for tt in range(8):
                    ts_ = slice(tt * 128, (tt + 1) * 128)
                    p = ps()
                    for k in range(KC):
                        src = yaT if k < 16 else ybT
                        MM(p[:, :], src[:, k % 16, ts_], Wo[:, k, :], k == 0, k == KC - 1, [src.b, Wo.b], [p.b])
                    xs = xs_[tt % 2]
                    xo = x2t[tt % 2]
                    S.dma("sp", xs[:], x_own[tok0 + tt * 128:tok0 + (tt + 1) * 128, ds_], writes=[xs.b])
                    TT("dve", xo[:], p[:, :], g1bc[:, ds_], ALU.mult, [p.b, g1bc.b], [xo.b])
                    TT("pool", xo[:], xo[:], xs[:], ALU.add, [xo.b, xs.b], [xo.b])
                    S.dma("act", x2_d.ap()[tok0 + tt * 128:tok0 + (tt + 1) * 128, ds_], xo[:], reads=[xo.b], writes=[b_x2], keep_w=True)
            S.barrier()
        MID.close()


    if stop == "O":
        dx = dbg_out("x2", [NOB_RUN * TOWN, D])
        S.dma("sp", dx, x2_d.ap()[0:NOB_RUN * TOWN, :], reads=[b_x2], writes=[Buf("dbgx2")])
        return finish()

    NBE = TC // HALF
    if nob:
        NBE = 2 * nob
    if nblk:
        NBE = 1
    with ExitStack() as ph:
        def pt(name, shape, dt=F32, st=None):
            return Tl(nc, st or ph, name, shape, dt)
        h2T = pt("h2T", [128, KC, HALF], BF16)
        acc = pt("acc", [128, 4, D])
        Gm = pt("Gm", [128, 4, NEO])
        for tb in range(NBE):
            with ExitStack() as rs:
                h2f = pt("h2f", [128, KC, 128], F32, rs)
                rw = pt("rw", [128, KC, NE], F32, rs)
                rbb = pt("rbb", [128, NE], F32, rs)
                xt = pt("xtM", [128, D], F32, rs)
                sc = pt("sc", [128, NE], F32, rs); sel = pt("sel", [128, NE], F32, rs); selm = pt("selm_", [128, NE], F32, rs)
                m8 = pt("m8", [128, 8, 8], F32, rs); gs = pt("gs", [128, 8], F32, rs); g8 = pt("g8", [128, 8], F32, rs)
                gmask = pt("gmask", [128, 8], F32, rs); goff = pt("goff", [128, 8], F32, rs); t8 = pt("t8", [128, 8], F32, rs)
                smask = pt("smask", [128, NE], F32, rs); gsum = pt("gsum", [128, 1], F32, rs)
                S.dma("sp", rw[:], rw_d.rearrange("(k p) e -> p k e", p=128), writes=[rw.b])
                bc_row(rbb[:], rb_d, [], [rbb.b])
                for tt in range(4):
                    r0 = tb * HALF + tt * 128
                    S.dma("sp", xt[:], x2_d.ap()[r0:r0 + 128, :], reads=[b_x2], writes=[xt.b])
                    norm_T(xt, s2, sh2, modT.b,
                           [(lambda c, tt=tt: h2T[:, c, tt * 128:(tt + 1) * 128], h2T.b),
                            (lambda c: h2f[:, c, :], h2f.b)])
                    p = ps()
                    for k in range(KC):
                        MM(p[:, 0:NE], h2f[:, k, :], rw[:, k, :], k == 0, k == KC - 1, [h2f.b, rw.b], [p.b])
                    ACT(sc[:], p[:, 0:NE], AF.Sigmoid, [p.b], [sc.b])
                    TT("dve", sel[:], sc[:], rbb[:], ALU.add, [sc.b, rbb.b], [sel.b])
                    for g in range(8):
                        S.op("dve", lambda e, g=g: e.max(out=m8[:, g, :], in_=sel[:, g * 16:(g + 1) * 16]), [sel.b], [m8.b])
                    TT("dve", gs[:], m8[:, :, 0], m8[:, :, 1], ALU.add, [m8.b], [gs.b])
                    S.op("dve", lambda e: e.max(out=g8[:], in_=gs[:]), [gs.b], [g8.b])
                    TS("dve", gmask[:], gs[:], g8[:, 3:4], None, ALU.is_ge, None, [gs.b, g8.b], [gmask.b])
                    TS("dve", goff[:], gmask[:], 4.0, -4.0, ALU.mult, ALU.add, [gmask.b], [goff.b])
                    for g in range(8):
                        gsl = slice(g * 16, (g + 1) * 16)
                        TS("dve", selm[:, gsl], sel[:, gsl], gmask[:, g:g + 1], goff[:, g:g + 1], ALU.mult, ALU.add,
                           [sel.b, gmask.b, goff.b], [selm.b])
                    S.op("dve", lambda e: e.max(out=t8[:], in_=selm[:]), [selm.b], [t8.b])
                    TS("dve", smask[:], selm[:], t8[:, 7:8], None, ALU.is_ge, None, [selm.b, t8.b], [smask.b])
                    TT("dve", smask[:], smask[:], sc[:], ALU.mult, [smask.b, sc.b], [smask.b])
                    S.op("dve", lambda e: e.reduce_sum(out=gsum[:], in_=smask[:], axis=AX.X), [smask.b], [gsum.b])
                    RECIP(gsum[:], gsum[:], [gsum.b], [gsum.b])
                    TS("dve", Gm[:, tt, 0:NEO - 1], smask[:, 0:NEO - 1], gsum[:, 0:1], 2.5, ALU.mult, ALU.mult,
                       [smask.b, gsum.b], [Gm.b])
                    MEMSET("pool", Gm[:, tt, NEO - 1:NEO], 1.0, [Gm.b])
                S.barrier()
            if stop == "R":
                dg = dbg_out("Gm", [128, 4 * NEO])
                S.dma("sp", dg, Gm[:].rearrange("p a b -> p (a b)"), reads=[Gm.b], writes=[Buf("dbgg")])
                return finish()
            with ExitStack() as xs_:
                Wg = pt("Wg", [128, KC, DE], BF16, xs_); Wu_ = pt("Wu_", [128, KC, DE], BF16, xs_)
                Wd = pt("Wd", [128, 3, D], BF16, xs_)
                stg = [pt("stgE%d" % i, [128, 2048], F32, xs_) for i in range(2)]
                hb = pt("hb", [128, 3, HALF], BF16, xs_); sl = pt("sl", [128, HALF], F32, xs_)
                for e_ in range(NEO):
                    load_w_bf16(lambda c: (Wg[:, c, :], Wg.b), lambda c, e_=e_: ewg_d[e_, c * 128:(c + 1) * 128, :], KC, DE, stg)
                    load_w_bf16(lambda c: (Wu_[:, c, :], Wu_.b), lambda c, e_=e_: ewu_d[e_, c * 128:(c + 1) * 128, :], KC, DE, stg)
                    load_w_bf16(lambda c: (Wd[:, c // 2, (c % 2) * 2048:(c % 2 + 1) * 2048], Wd.b),
                                lambda c, e_=e_: ewd_d[e_, (c // 2) * 128:(c // 2 + 1) * 128, (c % 2) * 2048:(c % 2 + 1) * 2048],
                                6, 2048, stg)
                    for f in range(3):
                        fs = slice(f * 128, (f + 1) * 128)
                        pg, pu = ps(), ps()
                        for k in range(KC):
                            MM(pg[:, :], Wg[:, k, fs], h2T[:, k, :], k == 0, k == KC - 1, [Wg.b, h2T.b], [pg.b])
                        for k in range(KC):
                            MM(pu[:, :], Wu_[:, k, fs], h2T[:, k, :], k == 0, k == KC - 1, [Wu_.b, h2T.b], [pu.b])
                        ACT(sl[:], pg[:, :], AF.Silu, [pg.b], [sl.b])
                        TT("dve", hb[:, f, :], pu[:, :], sl[:], ALU.mult, [pu.b, sl.b], [hb.b])
                    for tt in range(4):
                        for db in range(8):
                            p = ps()
                            for f in range(3):
                                MM(p[:, :], hb[:, f, tt * 128:(tt + 1) * 128], Wd[:, f, db * 512:(db + 1) * 512], f == 0, f == 2,
                                   [hb.b, Wd.b], [p.b])
                            dsl = slice(db * 512, (db + 1) * 512)
                            if e_ == 0:
                                TS("dve", acc[:, tt, dsl], p[:, :], Gm[:, tt, e_:e_ + 1], None, ALU.mult, None,
                                   [p.b, Gm.b], [acc.b])
                            else:
                                STT(acc[:, tt, dsl], p[:, :], Gm[:, tt, e_:e_ + 1], acc[:, tt, dsl], ALU.mult, ALU.add,
                                    [p.b, Gm.b, acc.b], [acc.b])
                S.barrier()
            with ExitStack() as fs_:
                g2bc = pt("g2bc", [128, D], F32, fs_); fbc = pt("fbc", [128, D], F32, fs_); xt = pt("xtF", [128, D], F32, fs_)
                bc_row(g2bc[:], agout.ap()[160:192, :].rearrange("(o a) b -> o (a b)", o=1), [b_agout], [g2bc.b])
                bc_row(fbc[:], fing_d, [], [fbc.b])
                for tt in range(4):
                    r0 = tb * HALF + tt * 128
                    S.dma("sp", xt[:], x2_d.ap()[r0:r0 + 128, :], reads=[b_x2], writes=[xt.b])
                    if stop == "E":
                        S.dma("sp", dbg_out("acc%d" % tt, [128, D]), acc[:, tt, :], reads=[acc.b], writes=[Buf("dbgacc")])
                    TT("pool", acc[:, tt, :], acc[:, tt, :], g2bc[:], ALU.mult, [acc.b, g2bc.b], [acc.b])
                    TT("dve", xt[:], xt[:], acc[:, tt, :], ALU.add, [xt.b, acc.b], [xt.b])
                    ACT(junk[:], xt[:], AF.Square, [xt.b], [junk.b, ssq.b], accum_out=ssq[:])
                    TS("dve", rstd[:], ssq[:], 1.0 / D, NORM_EPS, ALU.mult, ALU.add, [ssq.b], [rstd.b])
                    ACT(rstd[:], rstd[:], AF.Sqrt, [rstd.b], [rstd.b])
                    RECIP(rstd[:], rstd[:], [rstd.b], [rstd.b])
                    STT(xt[:], xt[:], rstd[:, 0:1], fbc[:], ALU.mult, ALU.mult, [xt.b, rstd.b, fbc.b], [xt.b])
                    S.dma("sp", out_d[r0:r0 + 128, :], xt[:], reads=[xt.b], writes=[b_out], keep_w=True)
                S.barrier()
    return finish()


def _consts():
    ident = np.eye(128, dtype=np.float32)
    bones = np.zeros((128, 128), np.float32)
    bones[:64, :64] = 1.0
    bones[64:, 64:] = 1.0
    s = np.arange(128)[:, None]
    t = np.arange(128)[None, :]
    mS = (s < t).astype(np.float32)
    mI = (s <= t).astype(np.float32)
    mask4 = np.concatenate([mS, mI, mS, mI], axis=1)
    mL = (t < s).astype(np.float32)
    maskl = np.concatenate([mL, mL], axis=1)
    i64s = np.concatenate([np.eye(64, dtype=np.float32)] * 2, axis=0)
    return dict(ident=ident, bones=bones, mask4=mask4, maskl=maskl, i64s=i64s)


_NC_CACHE = {}


def make_in_maps(x, c, mod_w, mod_b, norm1_g, norm2_g, w_in, w_out,
           a_ln_g, a_ln_b, a_spatial_w, a_spatial_b,
           b_shift_mu, b_decay_up, b_decay_base, b_iclr_up, b_iclr_base, b_gate_up,
           b_kk_scale, b_ka_scale, b_bonus, b_gn_g, b_gn_b,
           router_w, router_bias, exp_w_gate, exp_w_up, exp_w_down,
           sh_w_gate, sh_w_up, sh_w_down, final_g, _names=None):
    f = lambda a: np.ascontiguousarray(np.asarray(a, dtype=np.float32))
    x = f(x)[0]
    c = f(c)[0]
    mod_w, mod_b = f(mod_w)[0], f(mod_b)[0]
    w_in, w_out = f(w_in)[0], f(w_out)[0]
    mu = f(b_shift_mu)[0]
    dup, iup, gup = f(b_decay_up)[0], f(b_iclr_up)[0], f(b_gate_up)[0]
    BO = 2 * A_W
    consts = _consts()
    want_e = _names is None or "ewg" in _names
    if want_e:
        ewg_, ewu_, ewd_ = f(exp_w_gate)[0], f(exp_w_up)[0], f(exp_w_down)[0]
        shg, shu, shd = f(sh_w_gate), f(sh_w_up), f(sh_w_down)
    rw_full, rb_full = f(router_w)[0], f(router_bias)[0]
    pad32 = lambda a: np.concatenate([a, np.zeros((a.shape[0], 128 - a.shape[1]), np.float32)], axis=1)
    def b_cols(i):
        return np.concatenate([
            w_in[:, BO + i * 256:BO + (i + 1) * 256],
            w_in[:, BO + B_W + i * 256:BO + B_W + (i + 1) * 256],
            w_in[:, BO + 2 * B_W + i * 256:BO + 2 * B_W + (i + 1) * 256],
            pad32(w_in[:, BO + 3 * B_W:BO + 3 * B_W + DLORA]),
            pad32(w_in[:, BO + 3 * B_W + DLORA:BO + 3 * B_W + 2 * DLORA]),
            w_in[:, BO + 3 * B_W + 2 * DLORA:]], axis=1)

    def mu_rows(i):
        z32 = np.zeros(32, np.float32)
        return np.concatenate([mu[i * 256:(i + 1) * 256], mu[B_W + i * 256:B_W + (i + 1) * 256],
                               mu[2 * B_W + i * 256:2 * B_W + (i + 1) * 256],
                               mu[3 * B_W:3 * B_W + DLORA], z32, mu[3 * B_W + DLORA:3 * B_W + 2 * DLORA], z32,
                               mu[3 * B_W + 2 * DLORA:]]).reshape(10, 128)

    def vec_rows(i):
        hs = slice(i * 256, (i + 1) * 256)
        return np.concatenate([f(b_decay_base)[0][hs], f(b_iclr_base)[0][hs], f(b_kk_scale)[0][hs],
                               f(b_ka_scale)[0][hs], f(b_bonus)[0].reshape(-1)[hs], f(b_gn_g)[0][hs],
                               f(b_gn_b)[0][hs], np.zeros(256, np.float32)]).reshape(16, 128)

    m = dict(
        x=x, c=c.reshape(KC, 128), mod_w=mod_w, mod_b=mod_b.reshape(1, NMOD * D),
        n1g=f(norm1_g)[0].reshape(KC, 128), n2g=f(norm2_g)[0].reshape(KC, 128), fing=f(final_g).reshape(1, D),
        w_in_a=np.ascontiguousarray(w_in[:, :BO]),
        w_in_b=np.ascontiguousarray(np.stack([b_cols(i) for i in range(NHG)])),
        mu=np.ascontiguousarray(np.stack([mu_rows(i) for i in range(NHG)])),
        w_out=w_out,
        alng=f(a_ln_g)[0].reshape(1, A_W), alnb=f(a_ln_b)[0].reshape(1, A_W),
        spw=f(a_spatial_w)[0], spb=f(a_spatial_b)[0].reshape(1, A_W),
        dup=np.ascontiguousarray(np.stack([dup[:, i * 256:(i + 1) * 256] for i in range(NHG)])),
        iup=np.ascontiguousarray(np.stack([iup[:, i * 256:(i + 1) * 256] for i in range(NHG)])),
        gup=np.ascontiguousarray(np.stack([gup[:, i * 256:(i + 1) * 256] for i in range(NHG)])),
        vecs=np.ascontiguousarray(np.stack([vec_rows(i) for i in range(NHG)])),
        rw=rw_full, rb=rb_full.reshape(1, NE), **consts)
    if want_e:
        m["ewg"] = np.concatenate([ewg_, shg], axis=0)
        m["ewu"] = np.concatenate([ewu_, shu], axis=0)
        m["ewd"] = np.concatenate([ewd_, shd], axis=0)
    if _names is not None:
        m = {k: v for k, v in m.items() if k in _names}
    in_maps = []
    for i in range(NCORES):
        mi = dict(m)
        if _names is None or "x_own" in _names:
            mi["x_own"] = np.ascontiguousarray(x[i * TC:(i + 1) * TC])
            cs = np.zeros((128, NCORES), np.float32)
            cs[:, i] = 1.0
            mi["csel"] = cs
        in_maps.append(mi)
    return in_maps


def kernel(**inputs):
    in_maps = make_in_maps(**inputs)
    if "nc" not in _NC_CACHE:
        _NC_CACHE["nc"] = build_nc()
    res = run_bass_kernel_spmd(_NC_CACHE["nc"], in_maps, core_ids=list(range(NCORES)))
    out = np.concatenate([np.asarray(r["out"], dtype=np.float32) for r in res.results], axis=0)
    return out.reshape(1, T, D)
```

```python
import numpy as np
from contextlib import ExitStack
import concourse.bass as bass
import concourse.mybir as mybir
from concourse.bass_utils import run_bass_kernel_spmd

F32 = mybir.dt.float32
BF16 = mybir.dt.bfloat16
I32 = mybir.dt.int32
AF = mybir.ActivationFunctionType
ALU = mybir.AluOpType
AX = mybir.AxisListType

NCORES = 8
TC = 8192 // NCORES
NHG = 8
NOB = 8 // NCORES
D = 4096
KC = D // 128
T = 8192
TOWN = 1024
NMOD = 6
A_W = 2048
B_W = 2048
HD = 64
NH = 32
DLORA = 96
GLORA = 256
B_COLS = 3 * B_W + 2 * DLORA + GLORA
IN_COLS = 2 * A_W + B_COLS
NE = 128
DE = 384
NORM_EPS = 1e-6
LN_EPS = 1e-5
GN_EPS = 64e-5
EXPM05 = float(np.exp(-0.5))

ENGS = ("pe", "act", "dve", "pool", "sp")


class Buf:
    __slots__ = ("name", "w", "r", "dsem", "dcnt")

    def __init__(self, name):
        self.name = name
        self.w = []
        self.r = []
        self.dsem = None
        self.dcnt = 0


class Sched:
    def __init__(self, nc, es):
        self.nc = nc
        self.es = es
        self.q = {e: [] for e in ENGS}
        self.cnt = {e: 0 for e in ENGS}
        self.sem = {e: es.enter_context(nc.semaphore("s_" + e)) for e in ENGS}
        self.waited = {e: {} for e in ENGS}
        self.semobj = {e: self.sem[e] for e in ENGS}
        self.dsems = {}
        self.ninst = 0

    def _dsem(self, b):
        rec = self.dsems.get(b.name)
        if rec is None:
            sem = self.es.enter_context(self.nc.semaphore("d%d_%s" % (len(self.dsems), b.name)))
            rec = {"sem": sem, "cnt": 0}
            self.dsems[b.name] = rec
            self.semobj["d:" + b.name] = sem
        return rec

    def _wait(self, eng, tok):
        kind, key, val = tok
        if kind == "eng" and key == eng and eng == "pe":
            return
        cur = self.waited[eng].get(key, 0)
        if cur >= val:
            return
        self.waited[eng][key] = val
        sem = self.semobj[key]
        self.q[eng].append(lambda e, sem=sem, val=val: e.wait_ge(sem, val))

    def _deps(self, eng, reads, writes):
        for b in reads:
            for tok in b.w:
                self._wait(eng, tok)
        for b in writes:
            for tok in b.w:
                self._wait(eng, tok)
            for tok in b.r:
                self._wait(eng, tok)

    def op(self, eng, fn, reads=(), writes=()):
        if getattr(self, "limit", None) and self.ninst >= self.limit:
            return
        self._deps(eng, reads, writes)
        self.cnt[eng] += 1
        n = self.cnt[eng]
        sem = self.sem[eng]
        self.q[eng].append(lambda e, fn=fn, sem=sem: fn(e).then_inc(sem, 1))
        tok = ("eng", eng, n)
        for b in reads:
            b.r.append(tok)
        for b in writes:
            b.w = [tok]
            b.r = []
        self.ninst += 1

    def dma(self, q, out, in_, reads=(), writes=(), owner=None, keep_w=False, **kw):
        if getattr(self, "limit", None) and self.ninst >= self.limit and not kw.pop("force", False):
            return
        kw.pop("force", None)
        if owner is None:
            owner = writes[0]
        rec = self._dsem(owner)
        sem = rec["sem"]
        okey = "d:" + owner.name
        for b in reads:
            for tok in b.w:
                self._wait(q, tok)
        for b in writes:
            if not keep_w:
                for tok in b.w:
                    self._wait(q, tok)
            for tok in b.r:
                self._wait(q, tok)
        rec["cnt"] += 16
        val = rec["cnt"]
        self.q[q].append(lambda e, out=out, in_=in_, sem=sem, kw=kw:
                         e.dma_start(out=out, in_=in_, **kw).then_inc(sem, 16))
        tok = ("dma", okey, val)
        for b in reads:
            b.r.append(tok)
        for b in writes:
            if keep_w:
                b.w = [t for t in b.w if not (t[0] == "dma" and t[1] == okey)] + [tok]
            else:
                b.w = [tok]
            b.r = []
        self.ninst += 1

    def barrier(self):
        for e in ENGS:
            for f in ENGS:
                if f != e and self.cnt[f] > 0:
                    self._wait(e, ("eng", f, self.cnt[f]))
            for name, rec in self.dsems.items():
                if rec["cnt"] > 0:
                    self._wait(e, ("dma", "d:" + name, rec["cnt"]))

    def final_wait(self, eng="sp"):
        for f in ENGS:
            if f != eng and self.cnt[f] > 0:
                self._wait(eng, ("eng", f, self.cnt[f]))
        for name, rec in self.dsems.items():
            if rec["cnt"] > 0:
                self._wait(eng, ("dma", "d:" + name, rec["cnt"]))

    def emit(self):
        nc = self.nc
        with nc.Block() as block:
            @block.tensor
            def _(e):
                for t in self.q["pe"]:
                    t(e)

            @block.scalar
            def _(e):
                for t in self.q["act"]:
                    t(e)

            @block.vector
            def _(e):
                for t in self.q["dve"]:
                    t(e)

            @block.gpsimd
            def _(e):
                for t in self.q["pool"]:
                    t(e)

            @block.sync
            def _(e):
                for t in self.q["sp"]:
                    t(e)


class Tl:
    n = 0

    def __init__(self, nc, stack, name, shape, dt, psum=False):
        f = nc.psum_tensor if psum else nc.sbuf_tensor
        Tl.n += 1
        self.t = stack.enter_context(f("t%d_%s" % (Tl.n, name), list(shape), dt))
        self.b = Buf(name)

    def __getitem__(self, k):
        return self.t[k]


def build_nc(nblk=None, stop=None, nhg=None, nob=None, limit=None, fake_mod=False):
    nc = bass.Bass("TRN2", target_bir_lowering=False)
    es = ExitStack()
    S = Sched(nc, es)
    S.limit = limit

    in_names = []
    order = ["M", "B", "A", "S", "O", "R", "E", None]

    def din(name, shape, dt=F32, need=None):
        if need is not None and order.index(stop) < order.index(need):
            return None
        in_names.append(name)
        return nc.dram_tensor(name, list(shape), dt, kind="ExternalInput").ap()

    x_in = din("x", [T, D])
    x_own = din("x_own", [TC, D], need="A")
    csel_d = din("csel", [128, NCORES], need="A")
    c_in = din("c", [KC, 128])
    mod_w = None if fake_mod else din("mod_w", [D, NMOD * D])
    mod_b = din("mod_b", [1, NMOD * D])
    n1g_d = din("n1g", [KC, 128])
    n2g_d = din("n2g", [KC, 128])
    fing_d = din("fing", [1, D])
    w_in_a = din("w_in_a", [D, 2 * A_W], need="A")
    w_in_b = din("w_in_b", [NHG, D, 1280])
    mu_d = din("mu", [NHG, 10, 128])
    w_out_d = din("w_out", [D, D], need="O")
    alng_d = din("alng", [1, A_W])
    alnb_d = din("alnb", [1, A_W])
    spw_d = din("spw", [16, 128, 128])
    spb_d = din("spb", [1, A_W])
    dup_d = din("dup", [NHG, DLORA, 256])
    iup_d = din("iup", [NHG, DLORA, 256])
    gup_d = din("gup", [NHG, GLORA, 256])
    vecs_d = din("vecs", [NHG, 16, 128])
    rw_d = din("rw", [D, NE])
    rb_d = din("rb", [1, NE])
    NEO = NE + 1
    ewg_d = din("ewg", [NEO, D, DE], need="E")
    ewu_d = din("ewu", [NEO, D, DE], need="E")
    ewd_d = din("ewd", [NEO, DE, D], need="E")
    ident_d = din("ident", [128, 128])
    bones_d = din("bones", [128, 128])
    mask4_d = din("mask4", [128, 512])
    maskl_d = din("maskl", [128, 256])
    i64s_d = din("i64s", [128, 64])
    out_d = nc.dram_tensor("out", [TC, D], F32, kind="ExternalOutput").ap()

    agin = nc.dram_tensor("agin", [24, 128], F32)
    agout = nc.dram_tensor("agout", [192, 128], F32)
    ybin = nc.dram_tensor("ybin", [256, T], BF16)
    ybout = nc.dram_tensor("ybout", [2048, T], BF16)
    x2_d = nc.dram_tensor("x2s", [TC, D], F32)
    b_agin, b_agout, b_ybin, b_ybout, b_x2 = (Buf(n) for n in ("agin", "agout", "ybin", "ybout", "x2s"))
    b_out = Buf("out")
    xin = nc.dram_tensor("xin", [TOWN, D], F32)
    xall = nc.dram_tensor("xall", [T, D], F32)
    wina_in = nc.dram_tensor("wina_in", [D // NCORES, 2 * A_W], F32)
    wina = nc.dram_tensor("wina", [D, 2 * A_W], F32)
    wout_in = nc.dram_tensor("wout_in", [D // NCORES, D], F32)
    wout = nc.dram_tensor("wout", [D, D], F32)
    x2all = nc.dram_tensor("x2all", [T, D], F32)
    part_d = nc.dram_tensor("part", [T, D], F32)
    psum_d = nc.dram_tensor("partsum", [T, D], F32)
    b_xin, b_xall, b_wina_in, b_wina, b_wout_in, b_wout, b_x2all, b_part, b_psum = (
        Buf(n) for n in ("xin", "xall", "wina_in", "wina", "wout_in", "wout", "x2all", "part", "partsum"))
    dbg = {}

    def dbg_out(name, shape, dt=F32):
        dbg[name] = nc.dram_tensor("dbg_" + name, list(shape), dt, kind="ExternalOutput").ap()
        return dbg[name]

    def finish():
        S.final_wait("sp")
        S.emit()
        nc._in_names = in_names
        nc._dbg = sorted(dbg)
        print("instr per engine", S.cnt, "dma sems", len(S.dsems))
        return nc
    ccs = es.enter_context(nc.semaphore("ccs"))
    S.semobj["ccs"] = ccs
    cc_count = [0]

    def allgather(src, dst, bsrc, bdst, kind="AllGather"):
        S._deps("pool", [bsrc], [bdst])
        cc_count[0] += 1
        n = cc_count[0]
        op = ALU.add if kind == "AllReduce" else ALU.bypass
        S.q["pool"].append(lambda e: e.collective_compute(
            kind, op, replica_groups=[list(range(NCORES))],
            ins=[src.ap().opt()], outs=[dst.ap().opt()]).then_inc(ccs, 1))
        tok = ("cc", "ccs", n)
        bsrc.r.append(tok)
        bdst.w = [tok]
        bdst.r = []

    def ACT(out, in_, func, R, W, **kw):
        S.op("act", lambda e: e.activation(out=out, in_=in_, func=func, **kw), R, W)

    def TT(eng, out, a, b, op, R, W):
        S.op(eng, lambda e: e.tensor_tensor(out=out, in0=a, in1=b, op=op), R, W)

    def TS(eng, out, a, s1, s2, op0, op1, R, W):
        if op1 is None:
            S.op(eng, lambda e: e.tensor_scalar(out=out, in0=a, scalar1=s1, scalar2=None, op0=op0), R, W)
        else:
            S.op(eng, lambda e: e.tensor_scalar(out=out, in0=a, scalar1=s1, scalar2=s2, op0=op0, op1=op1), R, W)

    def STT(out, a, s, b, op0, op1, R, W):
        S.op("dve", lambda e: e.scalar_tensor_tensor(out=out, in0=a, scalar=s, in1=b, op0=op0, op1=op1), R, W)

    def MM(out, lhsT, rhs, start, stop, R, W):
        S.op("pe", lambda e: e.matmul(out, lhsT, rhs, start=start, stop=stop), R, W)

    def CP(eng, out, in_, R, W):
        if eng == "act":
            S.op("act", lambda e: e.copy(out=out, in_=in_), R, W)
        else:
            S.op(eng, lambda e: e.tensor_copy(out=out, in_=in_), R, W)

    def RECIP(out, in_, R, W):
        S.op("dve", lambda e: e.reciprocal(out=out, in_=in_), R, W)

    def MEMSET(eng, out, val, W):
        S.op(eng, lambda e: e.memset(out, val), [], W)

    P = ExitStack()
    es.enter_context(P)

    def tile(name, shape, dt=F32, stack=None):
        return Tl(nc, stack or P, name, shape, dt)

    PS = [Tl(nc, P, "ps%d" % i, [128, 512], F32, psum=True) for i in range(8)]
    psi = [0]

    def ps():
        psi[0] = (psi[0] + 1) % 8
        return PS[psi[0]]

    ident = tile("ident", [128, 128])
    bones = tile("bones", [128, 128])
    mask4 = tile("mask4", [128, 512])
    maskl = tile("maskl", [128, 256])
    i64s = tile("i64s", [128, 64])
    i2 = tile("i2", [128, 256])
    ones = tile("ones", [128, 128])
    S.dma("sp", ident[:], ident_d, writes=[ident.b])
    S.dma("sp", bones[:], bones_d, writes=[bones.b])
    S.dma("sp", mask4[:], mask4_d, writes=[mask4.b])
    S.dma("sp", maskl[:], maskl_d, writes=[maskl.b])
    S.dma("sp", i64s[:], i64s_d, writes=[i64s.b])
    S.dma("sp", i2[:, 0:128], ident_d, writes=[i2.b])
    S.dma("sp", i2[:, 128:256], ident_d, writes=[i2.b], keep_w=True)
    MEMSET("pool", ones[:], 1.0, [ones.b])

    def TR(out, in_, k, R, W):
        S.op("pe", lambda e: e.transpose(out, in_, ident[0:k, 0:k]), list(R) + [ident.b], W)

    def load_pl(name, dram_rows, k, stack=None):
        o = tile(name, [128, k], F32, stack)
        with ExitStack() as tmp:
            rows = Tl(nc, tmp, name + "_rows", [k, 128], F32)
            S.dma("sp", rows[:], dram_rows, writes=[rows.b])
            p = ps()
            TR(p[:, 0:k], rows[:], k, [rows.b], [p.b])
            CP("dve", o[:], p[:, 0:k], [p.b], [o.b])
            S.barrier()
        return o

    cT = load_pl("cT", c_in, KC)
    ACT(cT[:], cT[:], AF.Silu, [cT.b], [cT.b])
    n1g = load_pl("n1gT", n1g_d, KC)
    n2g = load_pl("n2gT", n2g_d, KC)
    modT = tile("modT", [128, 192])
    with ExitStack() as ph:
        mw = [Tl(nc, ph, "mw%d" % i, [128, 3072], F32) for i in range(3)]
        mrow = Tl(nc, ph, "mrow", [1, 3072], F32)
        mbrow = Tl(nc, ph, "mbrow", [1, 3072], F32)
        mrows = [Tl(nc, ph, "mrows%d" % i, [96, 128], F32) for i in range(2)]
        if fake_mod:
            MEMSET("pool", mrow[:], 0.1, [mrow.b])
            for gq in range(8):
                S.dma("sp", agout.ap()[gq * 24:(gq + 1) * 24, :].rearrange("(o a) b -> o (a b)", o=1), mrow[:],
                      reads=[mrow.b], writes=[b_agout], keep_w=True)
        for gq in range(0 if fake_mod else 8):
            gsl = slice(gq * 3072, (gq + 1) * 3072)
            S.dma("sp", mbrow[:], mod_b[:, gsl], writes=[mbrow.b])
            for k in range(KC):
                w = mw[k % 3]
                S.dma("sp", w[:], mod_w[k * 128:(k + 1) * 128, gsl], writes=[w.b])
                for b in range(6):
                    MM(PS[b][0:1, :], cT[:, k:k + 1], w[:, b * 512:(b + 1) * 512], k == 0, k == KC - 1,
                       [cT.b, w.b], [PS[b].b])
            for b in range(6):
                TT("dve", mrow[:, b * 512:(b + 1) * 512], PS[b][0:1, :], mbrow[:, b * 512:(b + 1) * 512], ALU.add,
                   [PS[b].b, mbrow.b], [mrow.b])
            S.dma("sp", agout.ap()[gq * 24:(gq + 1) * 24, :].rearrange("(o a) b -> o (a b)", o=1), mrow[:],
                  reads=[mrow.b], writes=[b_agout], keep_w=True)
        for i in range(2):
            S.dma("sp", mrows[i][:], agout.ap()[i * 96:(i + 1) * 96, :], reads=[b_agout], writes=[mrows[i].b])
            p = ps()
            TR(p[:, 0:96], mrows[i][:], 96, [mrows[i].b], [p.b])
            CP("dve", modT[:, i * 96:(i + 1) * 96], p[:, 0:96], [p.b], [modT.b])
        S.barrier()
    s1 = tile("s1", [128, KC])
    s2 = tile("s2", [128, KC])
    STT(s1[:], modT[:, 32:64], 1.0, n1g[:], ALU.add, ALU.mult, [modT.b, n1g.b], [s1.b])
    STT(s2[:], modT[:, 128:160], 1.0, n2g[:], ALU.add, ALU.mult, [modT.b, n2g.b], [s2.b])
    sh1 = modT[:, 0:32]
    sh2 = modT[:, 96:128]
    if stop == "M":
        S.dma("sp", dbg_out("modT", [128, 192]), modT[:], reads=[modT.b], writes=[Buf("dbgm")])
        return finish()

    def bc_row(dst, src_row_ap, R, W):
        S.dma("sp", dst, src_row_ap.partition_broadcast(128).squeeze(1), reads=R, writes=W)

    junk = tile("junk", [128, D], BF16)
    ssq = tile("ssq", [128, 1])
    rstd = tile("rstd", [128, 1])

    def norm_T(xt, sc, sh, shb, dsts):
        ACT(junk[:], xt[:], AF.Square, [xt.b], [junk.b, ssq.b], accum_out=ssq[:])
        TS("dve", rstd[:], ssq[:], 1.0 / D, NORM_EPS, ALU.mult, ALU.add, [ssq.b], [rstd.b])
        ACT(rstd[:], rstd[:], AF.Sqrt, [rstd.b], [rstd.b])
        RECIP(rstd[:], rstd[:], [rstd.b], [rstd.b])
        TS("dve", xt[:], xt[:], rstd[:, 0:1], None, ALU.mult, None, [xt.b, rstd.b], [xt.b])
        for c4 in range(KC // 4):
            p = ps()
            for j in range(4):
                c = c4 * 4 + j
                TR(p[:, j * 128:(j + 1) * 128], xt[:, c * 128:(c + 1) * 128], 128, [xt.b], [p.b])
            for j in range(4):
                c = c4 * 4 + j
                for fn, b in dsts:
                    ACT(fn(c), p[:, j * 128:(j + 1) * 128], AF.Identity, [p.b, sc.b, shb], [b],
                        scale=sc[:, c:c + 1], bias=sh[:, c:c + 1])

    def load_w_bf16(dst_fn, src_rows_fn, nchunks, ncols, stg, R_extra=()):
        for c in range(nchunks):
            st = stg[c % len(stg)]
            S.dma("sp", st[:, 0:ncols], src_rows_fn(c), reads=list(R_extra), writes=[st.b])
            ap, b = dst_fn(c)
            CP("pool", ap, st[:, 0:ncols], [st.b], [b])

    NHG_RUN = nhg or NHG
    for hg in range(NHG_RUN):
        C0 = EXPM05
        BLK = 256
        NBLK = nblk or (T // BLK)
        with ExitStack() as ph:
            def pt(name, shape, dt=F32):
                return Tl(nc, ph, name, shape, dt)
            vecs = load_pl("vecsT", vecs_d[hg], 16, ph)
            muT = load_pl("muT", mu_d[hg], 10, ph)
            omka = pt("omka", [128, 2])
            TS("dve", omka[:], vecs[:, 6:8], -1.0, 1.0, ALU.mult, ALU.add, [vecs.b], [omka.b])
            WB = pt("WB", [128, KC, 1280], BF16)
            dupw = pt("dupw", [DLORA, 256], BF16)
            iupw = pt("iupw", [DLORA, 256], BF16)
            gupw = pt("gupw", [128, 2, 256], BF16)
            with ExitStack() as tmp:
                stg = [Tl(nc, tmp, "stgB%d" % i, [128, 1280], F32) for i in range(2)]
                load_w_bf16(lambda c: (WB[:, c, :], WB.b), lambda c: w_in_b[hg, c * 128:(c + 1) * 128, :], KC, 1280, stg)
                S.dma("sp", stg[0][0:DLORA, 0:256], dup_d[hg], writes=[stg[0].b])
                CP("pool", dupw[:], stg[0][0:DLORA, 0:256], [stg[0].b], [dupw.b])
                S.dma("sp", stg[1][0:DLORA, 0:256], iup_d[hg], writes=[stg[1].b])
                CP("pool", iupw[:], stg[1][0:DLORA, 0:256], [stg[1].b], [iupw.b])
                for j in range(2):
                    S.dma("sp", stg[j][:, 0:256], gup_d[hg, j * 128:(j + 1) * 128, :], writes=[stg[j].b])
                    CP("pool", gupw[:, j, :], stg[j][:, 0:256], [stg[j].b], [gupw.b])
                S.barrier()
            xts = [pt("xtB0", [128, D])] * 2
            h1T = pt("h1T", [128, KC, BLK], BF16)
            pB = [pt("pB%d" % i, [128, BLK + 1]) for i in range(10)]
            pS = [pt("pS%d" % i, [128, BLK]) for i in range(10)]
            tmpd = pt("tmpd", [128, BLK])
            twd = pt("twd", [128, BLK], BF16)
            adb = pt("adb", [128, BLK], BF16)
            sgd = pt("sgd", [128, 2, BLK], BF16)
            sg = pt("sg", [128, BLK]); av = pt("av", [128, BLK]); gate = [pt("gate%d" % i, [128, BLK]) for i in range(2)]
            sq = pt("sq", [128, BLK]); rinv = pt("rinv", [128, BLK]); kk = pt("kk", [128, BLK])
            tk = pt("tk", [128, BLK]); kmod = pt("kmod", [128, BLK]); bvec = pt("bvec", [128, BLK])
            rkb = pt("rkb", [128, BLK]); bterm = [pt("bterm%d" % i, [128, BLK]) for i in range(2)]
            Gc = pt("Gc", [128, BLK]); Gx = pt("Gx", [128, BLK]); nb = pt("nb", [128, 2]); PCt = pt("PCt", [128, 2])
            E1 = pt("E1", [128, BLK]); Em = pt("Em", [128, BLK]); Ep = pt("Ep", [128, BLK]); Ee = pt("Ee", [128, BLK])
            BKt = [pt("BKt%d" % i, [128, 2, BLK], BF16) for i in range(2)]
            ARt = [pt("ARt%d" % i, [128, 2, BLK], BF16) for i in range(2)]
            TM = [pt("TM%d" % i, [128, 4, BLK]) for i in range(2)]
            TOK = pt("TOK", [128, 4, 128], BF16)
            ABm = pt("ABm", [128, 512], BF16); AKm = pt("AKm", [128, 512], BF16)
            Lk = [pt("Lk%d" % i, [128, 256], BF16) for i in range(2)]
            Mk = [pt("Mk%d" % i, [128, 256], BF16) for i in range(2)]
            Tt = [pt("Tt%d" % i, [128, 256], BF16) for i in range(2)]
            Xs = pt("Xs", [128, 128], BF16)
            WU = pt("WU", [128, 256], BF16)
            QT = pt("QT", [128, 128], BF16)
            Mc = pt("Mc", [128, 64], BF16)
            NcT = pt("NcT", [128, 64])
            ST = [[pt("ST%d_%d" % (ct, i), [128, 64], BF16) for i in range(2)] for ct in range(2)]
            st6 = pt("st6", [128, 2, 6]); mv = pt("mv", [128, 2, 2]); grs = pt("grs", [128, 2])
            yn = pt("yn", [128, 128]); ybn = pt("ybn", [128, 128])
            ybo = [pt("ybo%d" % i, [128, BLK], BF16) for i in range(2)]
            for i in range(10):
                MEMSET("pool", pB[i][:, 0:1], 0.0, [pB[i].b])
            for ct in range(2):
                MEMSET("pool", ST[ct][0][:], 0.0, [ST[ct][0].b])
            stp = [0, 0]

            for blk in range(NBLK):
                for tt in range(BLK // 128):
                    xt = xts[tt % 2]
                    r0 = blk * BLK + tt * 128
                    S.dma("sp", xt[:], x_in[r0:r0 + 128, :], writes=[xt.b])
                    norm_T(xt, s1, sh1, modT.b,
                           [(lambda c, tt=tt: h1T[:, c, tt * 128:(tt + 1) * 128], h1T.b)])
                for ci in range(10):
                    p = ps()
                    for k in range(KC):
                        MM(p[:, 0:BLK], WB[:, k, ci * 128:(ci + 1) * 128], h1T[:, k, :], k == 0, k == KC - 1,
                           [WB.b, h1T.b], [p.b])
                    CP("act", pB[ci][:, 1:BLK + 1], p[:, 0:BLK], [p.b], [pB[ci].b])
                    TT("dve", tmpd[:], pB[ci][:, 0:BLK], pB[ci][:, 1:BLK + 1], ALU.subtract, [pB[ci].b], [tmpd.b])
                    STT(pS[ci][:], tmpd[:], muT[:, ci:ci + 1], pB[ci][:, 1:BLK + 1], ALU.mult, ALU.add,
                        [tmpd.b, muT.b, pB[ci].b], [pS[ci].b])
                    CP("pool", pB[ci][:, 0:1], pB[ci][:, BLK:BLK + 1], [pB[ci].b], [pB[ci].b])
                ACT(twd[0:DLORA, :], pS[6][0:DLORA, :], AF.Tanh, [pS[6].b], [twd.b])
                CP("pool", adb[0:DLORA, :], pS[7][0:DLORA, :], [pS[7].b], [adb.b])
                for j in range(2):
                    ACT(sgd[:, j, :], pS[8 + j][:], AF.Sigmoid, [pS[8 + j].b], [sgd.b])
                for ct in range(2):
                    cs = slice(ct * 128, (ct + 1) * 128)
                    r_, k_, v_ = pS[ct], pS[2 + ct], pS[4 + ct]
                    p = ps()
                    MM(p[:, 0:BLK], dupw[:, cs], twd[0:DLORA, :], True, True, [dupw.b, twd.b], [p.b])
                    ACT(sg[:], p[:, 0:BLK], AF.Sigmoid, [p.b, vecs.b], [sg.b], bias=vecs[:, 0 + ct:1 + ct])
                    p = ps()
                    MM(p[:, 0:BLK], iupw[:, cs], adb[0:DLORA, :], True, True, [iupw.b, adb.b], [p.b])
                    ACT(av[:], p[:, 0:BLK], AF.Sigmoid, [p.b, vecs.b], [av.b], bias=vecs[:, 2 + ct:3 + ct])
                    p = ps()
                    for j in range(2):
                        MM(p[:, 0:BLK], gupw[:, j, cs], sgd[:, j, :], j == 0, j == 1, [gupw.b, sgd.b], [p.b])
                    CP("act", gate[ct][:], p[:, 0:BLK], [p.b], [gate[ct].b])
                    ACT(sq[:], k_[:], AF.Square, [k_.b, vecs.b], [sq.b], scale=vecs[:, 4 + ct:5 + ct])
                    p = ps()
                    MM(p[:, 0:BLK], bones[:], sq[:], True, True, [bones.b, sq.b], [p.b])
                    ACT(rinv[:], p[:, 0:BLK], AF.Sqrt, [p.b], [rinv.b])
                    TS("dve", rinv[:], rinv[:], 1e-12, None, ALU.max, None, [rinv.b], [rinv.b])
                    RECIP(rinv[:], rinv[:], [rinv.b], [rinv.b])
                    STT(kk[:], k_[:], vecs[:, 4 + ct:5 + ct], rinv[:], ALU.mult, ALU.mult, [k_.b, vecs.b, rinv.b], [kk.b])
                    TS("dve", tk[:], av[:], vecs[:, 6 + ct:7 + ct], omka[:, ct:ct + 1], ALU.mult, ALU.add,
                       [av.b, vecs.b, omka.b], [tk.b])
                    TT("pool", kmod[:], k_[:], tk[:], ALU.mult, [k_.b, tk.b], [kmod.b])
                    TT("pool", bvec[:], kk[:], av[:], ALU.mult, [kk.b, av.b], [bvec.b])
                    STT(rkb[:], r_[:], vecs[:, 8 + ct:9 + ct], kmod[:], ALU.mult, ALU.mult, [r_.b, vecs.b, kmod.b], [rkb.b])
                    p = ps()
                    MM(p[:, 0:BLK], bones[:], rkb[:], True, True, [bones.b, rkb.b], [p.b])
                    TT("dve", bterm[ct][:], p[:, 0:BLK], v_[:], ALU.mult, [p.b, v_.b], [bterm[ct].b])
                    for u in range(2):
                        us = slice(u * 128, (u + 1) * 128)
                        S.op("dve", lambda e, us=us: e.tensor_tensor_scan(out=Gc[:, us], data0=ones[:, 0:128], data1=sg[:, us],
                                                                        initial=0.0, op0=ALU.mult, op1=ALU.add),
                             [ones.b, sg.b], [Gc.b])
                        TS("dve", nb[:, u:u + 1], Gc[:, u * 128 + 127:u * 128 + 128], -C0, None, ALU.mult, None, [Gc.b], [nb.b])
                    TT("pool", Gx[:], Gc[:], sg[:], ALU.subtract, [Gc.b, sg.b], [Gx.b])
                    ACT(E1[:], Gc[:], AF.Exp, [Gc.b], [E1.b], scale=-C0)
                    ACT(Em[:], Gc[:], AF.Exp, [Gc.b], [Em.b], scale=C0)
                    ACT(Ep[:], Gx[:], AF.Exp, [Gx.b], [Ep.b], scale=-C0)
                    for u in range(2):
                        us = slice(u * 128, (u + 1) * 128)
                        ACT(Ee[:, us], Gc[:, us], AF.Exp, [Gc.b, nb.b], [Ee.b], scale=C0, bias=nb[:, u:u + 1])
                    ACT(PCt[:], nb[:], AF.Exp, [nb.b], [PCt.b])
                    bk, ar, tm = BKt[ct], ARt[ct], TM[ct]
                    TT("dve", bk[:, 0, :], bvec[:], Em[:], ALU.mult, [bvec.b, Em.b], [bk.b])
                    TT("dve", bk[:, 1, :], kmod[:], Em[:], ALU.mult, [kmod.b, Em.b], [bk.b])
                    STT(tm[:, 0, :], kk[:], -1.0, Ep[:], ALU.mult, ALU.mult, [kk.b, Ep.b], [tm.b])
                    CP("pool", ar[:, 0, :], tm[:, 0, :], [tm.b], [ar.b])
                    TT("dve", ar[:, 1, :], r_[:], E1[:], ALU.mult, [r_.b, E1.b], [ar.b])
                    CP("pool", tm[:, 1, :], v_[:], [v_.b], [tm.b])
                    TT("pool", tm[:, 2, :], bvec[:], Ee[:], ALU.mult, [bvec.b, Ee.b], [tm.b])
                    TT("pool", tm[:, 3, :], kmod[:], Ee[:], ALU.mult, [kmod.b, Ee.b], [tm.b])

                    for u in range(2):
                        us = slice(u * 128, (u + 1) * 128)
                        p = ps()
                        for j in range(4):
                            TR(p[:, j * 128:(j + 1) * 128], tm[:, j, us], 128, [tm.b], [p.b])
                        CP("act", TOK[:].rearrange("p a b -> p (a b)"), p[:, :], [p.b], [TOK.b])
                        for h in range(2):
                            hp = slice(h * 64, (h + 1) * 64)
                            pa = ps()
                            MM(pa[:, 0:256], bk[hp, 0, us], ar[hp, :, us], True, True, [bk.b, ar.b], [pa.b])
                            MM(pa[:, 256:512], bk[hp, 1, us], ar[hp, :, us], True, True, [bk.b, ar.b], [pa.b])
                            pc = ps()
                            MM(pc[:, 0:128], ar[hp, 0, us], bk[hp, 0, us], True, True, [bk.b, ar.b], [pc.b])
                            TT("dve", ABm[:, h * 256:(h + 1) * 256], pa[:, 0:256], mask4[:, 0:256], ALU.mult, [pa.b, mask4.b], [ABm.b])
                            TT("dve", AKm[:, h * 256:(h + 1) * 256], pa[:, 256:512], mask4[:, 0:256], ALU.mult, [pa.b, mask4.b], [AKm.b])
                            TT("dve", Lk[0][:, h * 128:(h + 1) * 128], pc[:, 0:128], maskl[:, 0:128], ALU.mult, [pc.b, maskl.b], [Lk[0].b])
                        AB3 = ABm[:].rearrange("p (h x) -> p h x", h=2)
                        AK3 = AKm[:].rearrange("p (h x) -> p h x", h=2)
                        CP("pool", Mk[0][:].rearrange("p (h x) -> p h x", h=2), AB3[:, :, 0:128], [ABm.b], [Mk[0].b])
                        TT("pool", Tt[0][:], Mk[0][:], i2[:], ALU.add, [Mk[0].b, i2.b], [Tt[0].b])
                        px = ps()
                        for h in range(2):
                            MM(px[:, h * 64:(h + 1) * 64], AK3[:, h, 0:128], TOK[:, 1, h * 64:(h + 1) * 64], True, True,
                               [AKm.b, TOK.b], [px.b])
                        CP("act", Xs[:], px[:, 0:128], [px.b], [Xs.b])
                        ci_, ti_ = 0, 0
                        for it in range(6):
                            mk, lk, tcur = Mk[ci_], Lk[ci_], Tt[ti_]
                            mk2, lk2, tnew = Mk[1 - ci_], Lk[1 - ci_], Tt[1 - ti_]
                            if it < 5:
                                pm = ps()
                                for h in range(2):
                                    hs = slice(h * 128, (h + 1) * 128)
                                    MM(pm[:, hs], lk[:, hs], mk[:, hs], True, True, [lk.b, mk.b], [pm.b])
                            pl = ps()
                            for h in range(2):
                                hs = slice(h * 128, (h + 1) * 128)
                                MM(pl[:, hs], mk[:, hs], lk[:, hs], True, True, [lk.b, mk.b], [pl.b])
                            if it < 5:
                                CP("act", mk2[:], pm[:, 0:256], [pm.b], [mk2.b])
                            CP("dve", lk2[:], pl[:, 0:256], [pl.b], [lk2.b])
                            pt_ = ps()
                            for h in range(2):
                                hs = slice(h * 128, (h + 1) * 128)
                                MM(pt_[:, hs], lk2[:, hs], tcur[:, hs], True, True, [lk2.b, tcur.b], [pt_.b])
                            TT("dve", tnew[:], pt_[:, 0:256], tcur[:], ALU.add, [pt_.b, tcur.b], [tnew.b])
                            ci_, ti_ = 1 - ci_, 1 - ti_
                        tfin = Tt[ti_]
                        pw = ps()
                        for h in range(2):
                            hs = slice(h * 128, (h + 1) * 128)
                            MM(pw[:, h * 128:h * 128 + 64], tfin[:, hs], TOK[:, 0, h * 64:(h + 1) * 64], True, True,
                               [tfin.b, TOK.b], [pw.b])
                            MM(pw[:, h * 128 + 64:h * 128 + 128], tfin[:, hs], Xs[:, h * 64:(h + 1) * 64], True, True,
                               [tfin.b, Xs.b], [pw.b])
                        CP("act", WU[:], pw[:, 0:256], [pw.b], [WU.b])
                        pq = ps()
                        for h in range(2):
                            hp = slice(h * 64, (h + 1) * 64)
                            MM(pq[hp, 0:128], WU[:, h * 128:h * 128 + 64], AB3[:, h, 128:256], True, True, [WU.b, ABm.b], [pq.b])
                            MM(pq[hp, 128:192], WU[:, h * 128:h * 128 + 64], TOK[:, 2, h * 64:(h + 1) * 64], True, True,
                               [WU.b, TOK.b], [pq.b])
                        TT("dve", QT[:], pq[:, 0:128], ar[:, 1, us], ALU.add, [pq.b, ar.b], [QT.b])
                        STT(Mc[:], i64s[:], PCt[:, u:u + 1], pq[:, 128:192], ALU.mult, ALU.add, [i64s.b, PCt.b, pq.b], [Mc.b])
                        pn = ps()
                        for h in range(2):
                            hp = slice(h * 64, (h + 1) * 64)
                            MM(pn[hp, 0:64], TOK[:, 2, h * 64:(h + 1) * 64], WU[:, h * 128 + 64:h * 128 + 128], True, False,
                               [TOK.b, WU.b], [pn.b])
                            MM(pn[hp, 0:64], TOK[:, 3, h * 64:(h + 1) * 64], TOK[:, 1, h * 64:(h + 1) * 64], False, True,
                               [TOK.b], [pn.b])
                        CP("act", NcT[:], pn[:, 0:64], [pn.b], [NcT.b])
                        sold, snew = ST[ct][stp[ct]], ST[ct][1 - stp[ct]]
                        pys = [ps(), ps()]
                        for h in range(2):
                            hp = slice(h * 64, (h + 1) * 64)
                            py = pys[h]
                            yo = py[:, 0:64]
                            MM(yo, AB3[:, h, 128:256], WU[:, h * 128 + 64:h * 128 + 128], True, False, [ABm.b, WU.b], [py.b])
                            MM(yo, AK3[:, h, 128:256], TOK[:, 1, h * 64:(h + 1) * 64], False, False, [AKm.b, TOK.b], [py.b])
                            MM(yo, QT[hp, :], sold[hp, :], False, True, [QT.b, sold.b], [py.b])
                        for h in range(2):
                            hp = slice(h * 64, (h + 1) * 64)
                            pst = ps()
                            MM(pst[hp, 0:64], Mc[hp, :], sold[hp, :], True, True, [Mc.b, sold.b], [pst.b])
                            TT("dve", snew[hp, :], pst[hp, 0:64], NcT[hp, :], ALU.add, [pst.b, NcT.b, snew.b], [snew.b])
                        stp[ct] = 1 - stp[ct]
                        for h in range(2):
                            S.op("dve", lambda e, h=h, py=pys[h]: e.bn_stats(out=st6[:, h, :], in_=py[:, 0:64]), [pys[h].b], [st6.b])
                            S.op("dve", lambda e, h=h: e.bn_aggr(out=mv[:, h, :], in_=st6[:, h, :]), [st6.b], [mv.b])
                        TS("dve", grs[:], mv[:, :, 1], GN_EPS, None, ALU.add, None, [mv.b], [grs.b])
                        ACT(grs[:], grs[:], AF.Sqrt, [grs.b], [grs.b])
                        RECIP(grs[:], grs[:], [grs.b], [grs.b])
                        for h in range(2):
                            TS("dve", yn[:, h * 64:(h + 1) * 64], pys[h][:, 0:64], mv[:, h, 0:1], grs[:, h:h + 1],
                               ALU.subtract, ALU.mult, [pys[h].b, mv.b, grs.b, yn.b], [yn.b])
                        pz = ps()
                        TR(pz[:, 0:128], yn[:], 128, [yn.b], [pz.b])
                        ACT(ybn[:], pz[:, 0:128], AF.Identity, [pz.b, vecs.b], [ybn.b],
                            scale=vecs[:, 10 + ct:11 + ct], bias=vecs[:, 12 + ct:13 + ct])
                        TT("pool", ybn[:], ybn[:], bterm[ct][:, us], ALU.add, [ybn.b, bterm[ct].b], [ybn.b])
                        TT("pool", ybo[ct][:, us], ybn[:], gate[ct][:, us], ALU.mult, [ybn.b, gate[ct].b], [ybo[ct].b])
                    S.dma("act", ybout.ap()[hg * 256 + ct * 128:hg * 256 + (ct + 1) * 128, blk * BLK:(blk + 1) * BLK], ybo[ct][:],
                          reads=[ybo[ct].b], writes=[b_ybout], keep_w=True)
            S.barrier()

    if stop == "B":
        dy = dbg_out("yb", [NHG_RUN * 256, (nblk or 32) * 256], BF16)
        S.dma("sp", dy, ybout.ap()[0:NHG_RUN * 256, 0:(nblk or 32) * 256], reads=[b_ybout], writes=[Buf("dbgy")], force=True)
        return finish()

    NOB_RUN = nob or NOB
    for ob in range(NOB_RUN):
        tok0 = ob * TOWN
        MID = ExitStack()
        yaT = tile("yaT", [128, 16, TOWN], BF16, MID)
        HALF = 512
        with ExitStack() as ph:
            def pt(name, shape, dt=F32):
                return Tl(nc, ph, name, shape, dt)
            h1o = pt("h1o", [128, KC, HALF], BF16)
            vn = pt("vn", [128, 4, A_W], BF16)
            wsT = pt("wsT", [128, 16, 128], BF16)
            lng = pt("lng", [128, A_W]); lnb = pt("lnb", [128, A_W]); spb = pt("spb", [128, A_W])
            bc_row(lng[:], alng_d, [], [lng.b]); bc_row(lnb[:], alnb_d, [], [lnb.b]); bc_row(spb[:], spb_d, [], [spb.b])
            Wv = pt("Wv", [128, KC, 512], BF16)
            Wu = pt("Wu", [128, KC, 128], BF16)
            stg = [pt("stgA%d" % i, [128, 512]) for i in range(2)]
            vf = pt("vf", [128, 512]); vt = pt("vt", [128, 512])
            ast6 = pt("ast6", [128, 4, 6]); amv = pt("amv", [128, 4, 2]); ars = pt("ars", [128, 4])
            uT = pt("uT", [128, HALF]); mtmp = pt("mtmp", [128, 128])
            xts = [pt("xtA0", [128, D])] * 2
            for g in range(16):
                S.dma("sp", stg[g % 2][:, 0:128], spw_d[g], writes=[stg[g % 2].b])
                p = ps()
                TR(p[:, 0:128], stg[g % 2][:, 0:128], 128, [stg[g % 2].b], [p.b])
                TT("dve", wsT[:, g, :], p[:, 0:128], mask4[:, 128:256], ALU.mult, [p.b, mask4.b], [wsT.b])
            for hf in range(2):
                for tt in range(4):
                    xt = xts[tt % 2]
                    r0 = hf * HALF + tt * 128
                    S.dma("sp", xt[:], x_own[tok0 + r0:tok0 + r0 + 128, :], writes=[xt.b])
                    norm_T(xt, s1, sh1, modT.b, [(lambda c, tt=tt: h1o[:, c, tt * 128:(tt + 1) * 128], h1o.b)])
                for cb in range(4):
                    load_w_bf16(lambda c: (Wv[:, c, :], Wv.b),
                                lambda c, cb=cb: w_in_a[c * 128:(c + 1) * 128, A_W + cb * 512:A_W + (cb + 1) * 512], KC, 512, stg)
                    for tt in range(4):
                        p = ps()
                        for k in range(KC):
                            MM(p[:, :], h1o[:, k, tt * 128:(tt + 1) * 128], Wv[:, k, :], k == 0, k == KC - 1, [h1o.b, Wv.b], [p.b])
                        ACT(vf[:], p[:, :], AF.Gelu, [p.b], [vf.b])
                        for g in range(4):
                            gs = slice(g * 128, (g + 1) * 128)
                            S.op("dve", lambda e, g=g, gs=gs: e.bn_stats(out=ast6[:, g, :], in_=vf[:, gs]), [vf.b], [ast6.b])
                            S.op("dve", lambda e, g=g: e.bn_aggr(out=amv[:, g, :], in_=ast6[:, g, :]), [ast6.b], [amv.b])
                        TS("dve", ars[:], amv[:, :, 1], LN_EPS, None, ALU.add, None, [amv.b], [ars.b])
                        ACT(ars[:], ars[:], AF.Sqrt, [ars.b], [ars.b])
                        RECIP(ars[:], ars[:], [ars.b], [ars.b])
                        for g in range(4):
                            gs = slice(g * 128, (g + 1) * 128)
                            TS("dve", vt[:, gs], vf[:, gs], amv[:, g, 0:1], ars[:, g:g + 1], ALU.subtract, ALU.mult,
                               [vf.b, amv.b, ars.b], [vt.b])
                        cs = slice(cb * 512, (cb + 1) * 512)
                        TT("pool", vt[:], vt[:], lng[:, cs], ALU.mult, [vt.b, lng.b], [vt.b])
                        TT("pool", vn[:, tt, cs], vt[:], lnb[:, cs], ALU.add, [vt.b, lnb.b], [vn.b])
                for g in range(16):
                    load_w_bf16(lambda c: (Wu[:, c, :], Wu.b),
                                lambda c, g=g: w_in_a[c * 128:(c + 1) * 128, g * 128:(g + 1) * 128], KC, 128, stg)
                    p = ps()
                    for k in range(KC):
                        MM(p[:, :], Wu[:, k, :], h1o[:, k, :], k == 0, k == KC - 1, [Wu.b, h1o.b], [p.b])
                    ACT(uT[:], p[:, :], AF.Gelu, [p.b], [uT.b])
                    for tt in range(4):
                        p = ps()
                        MM(p[:, 0:128], vn[:, tt, g * 128:(g + 1) * 128], wsT[:, g, :], True, True, [vn.b, wsT.b], [p.b])
                        TT("dve", mtmp[:], p[:, 0:128], spb[:, g * 128:(g + 1) * 128], ALU.add, [p.b, spb.b], [mtmp.b])
                        c0 = hf * HALF + tt * 128
                        TT("pool", yaT[:, g, c0:c0 + 128], mtmp[:], uT[:, tt * 128:(tt + 1) * 128], ALU.mult,
                           [mtmp.b, uT.b], [yaT.b])
            S.barrier()

        ybT = tile("ybT", [128, 16, TOWN], BF16, MID)
        csel = tile("csel", [128, NCORES], F32, MID)
        S.dma("sp", csel[:], csel_d, writes=[csel.b])
        with ExitStack() as ysl:
            ycand = [Tl(nc, ysl, "ycand%d" % i, [128, TOWN], BF16) for i in range(2)]
            for ctg in range(16):
                for r in range(NCORES):
                    yc = ycand[r % 2]
                    S.dma("sp", yc[:], ybout.ap()[ctg * 128:(ctg + 1) * 128, r * TC + tok0:r * TC + tok0 + TOWN],
                          reads=[b_ybout], writes=[yc.b])
                    if r == 0:
                        TS("dve", ybT[:, ctg, :], yc[:], csel[:, 0:1], None, ALU.mult, None, [yc.b, csel.b], [ybT.b])
                    else:
                        STT(ybT[:, ctg, :], yc[:], csel[:, r:r + 1], ybT[:, ctg, :], ALU.mult, ALU.add,
                            [yc.b, csel.b, ybT.b], [ybT.b])
            S.barrier()

        with ExitStack() as ph:
            def pt(name, shape, dt=F32):
                return Tl(nc, ph, name, shape, dt)
            Wo = pt("Wo", [128, KC, 512], BF16)
            stg = [pt("stgO%d" % i, [128, 512]) for i in range(2)]
            g1bc = pt("g1bc", [128, D])
            xs_ = [pt("xsl%d" % i, [128, 512]) for i in range(2)]
            x2t = [pt("x2t%d" % i, [128, 512]) for i in range(2)]
            bc_row(g1bc[:], agout.ap()[64:96, :].rearrange("(o a) b -> o (a b)", o=1), [b_agout], [g1bc.b])
            for db in range(8):
                ds_ = slice(db * 512, (db + 1) * 512)
                load_w_bf16(lambda c: (Wo[:, c, :], Wo.b), lambda c, ds_=ds_: w_out_d[c * 128:(c + 1) * 128, ds_], KC, 512, stg)
                for tt in range(8):
                    ts_ = slice(tt * 128, (tt + 1) * 128)
                    p = ps()
                    for k in range(KC):
                        src = yaT if k < 16 else ybT
                        MM(p[:, :], src[:, k % 16, ts_], Wo[:, k, :], k == 0, k == KC - 1, [src.b, Wo.b], [p.b])
                    xs = xs_[tt % 2]
                    xo = x2t[tt % 2]
                    S.dma("sp", xs[:], x_own[tok0 + tt * 128:tok0 + (tt + 1) * 128, ds_], writes=[xs.b])
                    TT("dve", xo[:], p[:, :], g1bc[:, ds_], ALU.mult, [p.b, g1bc.b], [xo.b])
                    TT("pool", xo[:], xo[:], xs[:], ALU.add, [xo.b, xs.b], [xo.b])
                    S.dma("act", x2_d.ap()[tok0 + tt * 128:tok0 + (tt + 1) * 128, ds_], xo[:], reads=[xo.b], writes=[b_x2], keep_w=True)
            S.barrier()
        MID.close()


    if stop == "O":
        dx = dbg_out("x2", [NOB_RUN * TOWN, D])
        S.dma("sp", dx, x2_d.ap()[0:NOB_RUN * TOWN, :], reads=[b_x2], writes=[Buf("dbgx2")])
        return finish()

    NBE = TC // HALF
    if nob:
        NBE = 2 * nob
    if nblk:
        NBE = 1
    with ExitStack() as ph:
        def pt(name, shape, dt=F32, st=None):
            return Tl(nc, st or ph, name, shape, dt)
        h2T = pt("h2T", [128, KC, HALF], BF16)
        acc = pt("acc", [128, 4, D])
        Gm = pt("Gm", [128, 4, NEO])
        for tb in range(NBE):
            with ExitStack() as rs:
                h2f = pt("h2f", [128, KC, 128], F32, rs)
                rw = pt("rw", [128, KC, NE], F32, rs)
                rbb = pt("rbb", [128, NE], F32, rs)
                xt = pt("xtM", [128, D], F32, rs)
                sc = pt("sc", [128, NE], F32, rs); sel = pt("sel", [128, NE], F32, rs); selm = pt("selm_", [128, NE], F32, rs)
                m8 = pt("m8", [128, 8, 8], F32, rs); gs = pt("gs", [128, 8], F32, rs); g8 = pt("g8", [128, 8], F32, rs)
                gmask = pt("gmask", [128, 8], F32, rs); goff = pt("goff", [128, 8], F32, rs); t8 = pt("t8", [128, 8], F32, rs)
                smask = pt("smask", [128, NE], F32, rs); gsum = pt("gsum", [128, 1], F32, rs)
                S.dma("sp", rw[:], rw_d.rearrange("(k p) e -> p k e", p=128), writes=[rw.b])
                bc_row(rbb[:], rb_d, [], [rbb.b])
                for tt in range(4):
                    r0 = tb * HALF + tt * 128
                    S.dma("sp", xt[:], x2_d.ap()[r0:r0 + 128, :], reads=[b_x2], writes=[xt.b])
                    norm_T(xt, s2, sh2, modT.b,
                           [(lambda c, tt=tt: h2T[:, c, tt * 128:(tt + 1) * 128], h2T.b),
                            (lambda c: h2f[:, c, :], h2f.b)])
                    p = ps()
                    for k in range(KC):
                        MM(p[:, 0:NE], h2f[:, k, :], rw[:, k, :], k == 0, k == KC - 1, [h2f.b, rw.b], [p.b])
                    ACT(sc[:], p[:, 0:NE], AF.Sigmoid, [p.b], [sc.b])
                    TT("dve", sel[:], sc[:], rbb[:], ALU.add, [sc.b, rbb.b], [sel.b])
                    for g in range(8):
                        S.op("dve", lambda e, g=g: e.max(out=m8[:, g, :], in_=sel[:, g * 16:(g + 1) * 16]), [sel.b], [m8.b])
                    TT("dve", gs[:], m8[:, :, 0], m8[:, :, 1], ALU.add, [m8.b], [gs.b])
                    S.op("dve", lambda e: e.max(out=g8[:], in_=gs[:]), [gs.b], [g8.b])
                    TS("dve", gmask[:], gs[:], g8[:, 3:4], None, ALU.is_ge, None, [gs.b, g8.b], [gmask.b])
                    TS("dve", goff[:], gmask[:], 4.0, -4.0, ALU.mult, ALU.add, [gmask.b], [goff.b])
                    for g in range(8):
                        gsl = slice(g * 16, (g + 1) * 16)
                        TS("dve", selm[:, gsl], sel[:, gsl], gmask[:, g:g + 1], goff[:, g:g + 1], ALU.mult, ALU.add,
                           [sel.b, gmask.b, goff.b], [selm.b])
                    S.op("dve", lambda e: e.max(out=t8[:], in_=selm[:]), [selm.b], [t8.b])
                    TS("dve", smask[:], selm[:], t8[:, 7:8], None, ALU.is_ge, None, [selm.b, t8.b], [smask.b])
                    TT("dve", smask[:], smask[:], sc[:], ALU.mult, [smask.b, sc.b], [smask.b])
                    S.op("dve", lambda e: e.reduce_sum(out=gsum[:], in_=smask[:], axis=AX.X), [smask.b], [gsum.b])
                    RECIP(gsum[:], gsum[:], [gsum.b], [gsum.b])
                    TS("dve", Gm[:, tt, 0:NEO - 1], smask[:, 0:NEO - 1], gsum[:, 0:1], 2.5, ALU.mult, ALU.mult,
                       [smask.b, gsum.b], [Gm.b])
                    MEMSET("pool", Gm[:, tt, NEO - 1:NEO], 1.0, [Gm.b])
                S.barrier()
            if stop == "R":
                dg = dbg_out("Gm", [128, 4 * NEO])
                S.dma("sp", dg, Gm[:].rearrange("p a b -> p (a b)"), reads=[Gm.b], writes=[Buf("dbgg")])
                return finish()
            with ExitStack() as xs_:
                Wg = pt("Wg", [128, KC, DE], BF16, xs_); Wu_ = pt("Wu_", [128, KC, DE], BF16, xs_)
                Wd = pt("Wd", [128, 3, D], BF16, xs_)
                stg = [pt("stgE%d" % i, [128, 2048], F32, xs_) for i in range(2)]
                hb = pt("hb", [128, 3, HALF], BF16, xs_); sl = pt("sl", [128, HALF], F32, xs_)
                for e_ in range(NEO):
                    load_w_bf16(lambda c: (Wg[:, c, :], Wg.b), lambda c, e_=e_: ewg_d[e_, c * 128:(c + 1) * 128, :], KC, DE, stg)
                    load_w_bf16(lambda c: (Wu_[:, c, :], Wu_.b), lambda c, e_=e_: ewu_d[e_, c * 128:(c + 1) * 128, :], KC, DE, stg)
                    load_w_bf16(lambda c: (Wd[:, c // 2, (c % 2) * 2048:(c % 2 + 1) * 2048], Wd.b),
                                lambda c, e_=e_: ewd_d[e_, (c // 2) * 128:(c // 2 + 1) * 128, (c % 2) * 2048:(c % 2 + 1) * 2048],
                                6, 2048, stg)
                    for f in range(3):
                        fs = slice(f * 128, (f + 1) * 128)
                        pg, pu = ps(), ps()
                        for k in range(KC):
                            MM(pg[:, :], Wg[:, k, fs], h2T[:, k, :], k == 0, k == KC - 1, [Wg.b, h2T.b], [pg.b])
                        for k in range(KC):
                            MM(pu[:, :], Wu_[:, k, fs], h2T[:, k, :], k == 0, k == KC - 1, [Wu_.b, h2T.b], [pu.b])
                        ACT(sl[:], pg[:, :], AF.Silu, [pg.b], [sl.b])
                        TT("dve", hb[:, f, :], pu[:, :], sl[:], ALU.mult, [pu.b, sl.b], [hb.b])
                    for tt in range(4):
                        for db in range(8):
                            p = ps()
                            for f in range(3):
                                MM(p[:, :], hb[:, f, tt * 128:(tt + 1) * 128], Wd[:, f, db * 512:(db + 1) * 512], f == 0, f == 2,
                                   [hb.b, Wd.b], [p.b])
                            dsl = slice(db * 512, (db + 1) * 512)
                            if e_ == 0:
                                TS("dve", acc[:, tt, dsl], p[:, :], Gm[:, tt, e_:e_ + 1], None, ALU.mult, None,
                                   [p.b, Gm.b], [acc.b])
                            else:
                                STT(acc[:, tt, dsl], p[:, :], Gm[:, tt, e_:e_ + 1], acc[:, tt, dsl], ALU.mult, ALU.add,
                                    [p.b, Gm.b, acc.b], [acc.b])
                S.barrier()
            with ExitStack() as fs_:
                g2bc = pt("g2bc", [128, D], F32, fs_); fbc = pt("fbc", [128, D], F32, fs_); xt = pt("xtF", [128, D], F32, fs_)
                bc_row(g2bc[:], agout.ap()[160:192, :].rearrange("(o a) b -> o (a b)", o=1), [b_agout], [g2bc.b])
                bc_row(fbc[:], fing_d, [], [fbc.b])
                for tt in range(4):
                    r0 = tb * HALF + tt * 128
                    S.dma("sp", xt[:], x2_d.ap()[r0:r0 + 128, :], reads=[b_x2], writes=[xt.b])
                    if stop == "E":
                        S.dma("sp", dbg_out("acc%d" % tt, [128, D]), acc[:, tt, :], reads=[acc.b], writes=[Buf("dbgacc")])
                    TT("pool", acc[:, tt, :], acc[:, tt, :], g2bc[:], ALU.mult, [acc.b, g2bc.b], [acc.b])
                    TT("dve", xt[:], xt[:], acc[:, tt, :], ALU.add, [xt.b, acc.b], [xt.b])
                    ACT(junk[:], xt[:], AF.Square, [xt.b], [junk.b, ssq.b], accum_out=ssq[:])
                    TS("dve", rstd[:], ssq[:], 1.0 / D, NORM_EPS, ALU.mult, ALU.add, [ssq.b], [rstd.b])
                    ACT(rstd[:], rstd[:], AF.Sqrt, [rstd.b], [rstd.b])
                    RECIP(rstd[:], rstd[:], [rstd.b], [rstd.b])
                    STT(xt[:], xt[:], rstd[:, 0:1], fbc[:], ALU.mult, ALU.mult, [xt.b, rstd.b, fbc.b], [xt.b])
                    S.dma("sp", out_d[r0:r0 + 128, :], xt[:], reads=[xt.b], writes=[b_out], keep_w=True)
                S.barrier()
    return finish()


def _consts():
    ident = np.eye(128, dtype=np.float32)
    bones = np.zeros((128, 128), np.float32)
    bones[:64, :64] = 1.0
    bones[64:, 64:] = 1.0
    s = np.arange(128)[:, None]
    t = np.arange(128)[None, :]
    mS = (s < t).astype(np.float32)
    mI = (s <= t).astype(np.float32)
    mask4 = np.concatenate([mS, mI, mS, mI], axis=1)
    mL = (t < s).astype(np.float32)
    maskl = np.concatenate([mL, mL], axis=1)
    i64s = np.concatenate([np.eye(64, dtype=np.float32)] * 2, axis=0)
    return dict(ident=ident, bones=bones, mask4=mask4, maskl=maskl, i64s=i64s)


_NC_CACHE = {}


def make_in_maps(x, c, mod_w, mod_b, norm1_g, norm2_g, w_in, w_out,
           a_ln_g, a_ln_b, a_spatial_w, a_spatial_b,
           b_shift_mu, b_decay_up, b_decay_base, b_iclr_up, b_iclr_base, b_gate_up,
           b_kk_scale, b_ka_scale, b_bonus, b_gn_g, b_gn_b,
           router_w, router_bias, exp_w_gate, exp_w_up, exp_w_down,
           sh_w_gate, sh_w_up, sh_w_down, final_g, _names=None):
    f = lambda a: np.ascontiguousarray(np.asarray(a, dtype=np.float32))
    x = f(x)[0]
    c = f(c)[0]
    mod_w, mod_b = f(mod_w)[0], f(mod_b)[0]
    w_in, w_out = f(w_in)[0], f(w_out)[0]
    mu = f(b_shift_mu)[0]
    dup, iup, gup = f(b_decay_up)[0], f(b_iclr_up)[0], f(b_gate_up)[0]
    BO = 2 * A_W
    consts = _consts()
    want_e = _names is None or "ewg" in _names
    if want_e:
        ewg_, ewu_, ewd_ = f(exp_w_gate)[0], f(exp_w_up)[0], f(exp_w_down)[0]
        shg, shu, shd = f(sh_w_gate), f(sh_w_up), f(sh_w_down)
    rw_full, rb_full = f(router_w)[0], f(router_bias)[0]
    pad32 = lambda a: np.concatenate([a, np.zeros((a.shape[0], 128 - a.shape[1]), np.float32)], axis=1)
    def b_cols(i):
        return np.concatenate([
            w_in[:, BO + i * 256:BO + (i + 1) * 256],
            w_in[:, BO + B_W + i * 256:BO + B_W + (i + 1) * 256],
            w_in[:, BO + 2 * B_W + i * 256:BO + 2 * B_W + (i + 1) * 256],
            pad32(w_in[:, BO + 3 * B_W:BO + 3 * B_W + DLORA]),
            pad32(w_in[:, BO + 3 * B_W + DLORA:BO + 3 * B_W + 2 * DLORA]),
            w_in[:, BO + 3 * B_W + 2 * DLORA:]], axis=1)

    def mu_rows(i):
        z32 = np.zeros(32, np.float32)
        return np.concatenate([mu[i * 256:(i + 1) * 256], mu[B_W + i * 256:B_W + (i + 1) * 256],
                               mu[2 * B_W + i * 256:2 * B_W + (i + 1) * 256],
                               mu[3 * B_W:3 * B_W + DLORA], z32, mu[3 * B_W + DLORA:3 * B_W + 2 * DLORA], z32,
                               mu[3 * B_W + 2 * DLORA:]]).reshape(10, 128)

    def vec_rows(i):
        hs = slice(i * 256, (i + 1) * 256)
        return np.concatenate([f(b_decay_base)[0][hs], f(b_iclr_base)[0][hs], f(b_kk_scale)[0][hs],
                               f(b_ka_scale)[0][hs], f(b_bonus)[0].reshape(-1)[hs], f(b_gn_g)[0][hs],
                               f(b_gn_b)[0][hs], np.zeros(256, np.float32)]).reshape(16, 128)

    m = dict(
        x=x, c=c.reshape(KC, 128), mod_w=mod_w, mod_b=mod_b.reshape(1, NMOD * D),
        n1g=f(norm1_g)[0].reshape(KC, 128), n2g=f(norm2_g)[0].reshape(KC, 128), fing=f(final_g).reshape(1, D),
        w_in_a=np.ascontiguousarray(w_in[:, :BO]),
        w_in_b=np.ascontiguousarray(np.stack([b_cols(i) for i in range(NHG)])),
        mu=np.ascontiguousarray(np.stack([mu_rows(i) for i in range(NHG)])),
        w_out=w_out,
        alng=f(a_ln_g)[0].reshape(1, A_W), alnb=f(a_ln_b)[0].reshape(1, A_W),
        spw=f(a_spatial_w)[0], spb=f(a_spatial_b)[0].reshape(1, A_W),
        dup=np.ascontiguousarray(np.stack([dup[:, i * 256:(i + 1) * 256] for i in range(NHG)])),
        iup=np.ascontiguousarray(np.stack([iup[:, i * 256:(i + 1) * 256] for i in range(NHG)])),
        gup=np.ascontiguousarray(np.stack([gup[:, i * 256:(i + 1) * 256] for i in range(NHG)])),
        vecs=np.ascontiguousarray(np.stack([vec_rows(i) for i in range(NHG)])),
        rw=rw_full, rb=rb_full.reshape(1, NE), **consts)
    if want_e:
        m["ewg"] = np.concatenate([ewg_, shg], axis=0)
        m["ewu"] = np.concatenate([ewu_, shu], axis=0)
        m["ewd"] = np.concatenate([ewd_, shd], axis=0)
    if _names is not None:
        m = {k: v for k, v in m.items() if k in _names}
    in_maps = []
    for i in range(NCORES):
        mi = dict(m)
        if _names is None or "x_own" in _names:
            mi["x_own"] = np.ascontiguousarray(x[i * TC:(i + 1) * TC])
            cs = np.zeros((128, NCORES), np.float32)
            cs[:, i] = 1.0
            mi["csel"] = cs
        in_maps.append(mi)
    return in_maps


def kernel(**inputs):
    in_maps = make_in_maps(**inputs)
    if "nc" not in _NC_CACHE:
        _NC_CACHE["nc"] = build_nc()
    res = run_bass_kernel_spmd(_NC_CACHE["nc"], in_maps, core_ids=list(range(NCORES)))
    out = np.concatenate([np.asarray(r["out"], dtype=np.float32) for r in res.results], axis=0)
    return out.reshape(1, T, D)
```

```python
import numpy as np
from contextlib import ExitStack
import concourse.bass as bass
import concourse.mybir as mybir
from concourse.bass_utils import run_bass_kernel_spmd

F32 = mybir.dt.float32
BF16 = mybir.dt.bfloat16
I32 = mybir.dt.int32
AF = mybir.ActivationFunctionType
ALU = mybir.AluOpType
AX = mybir.AxisListType

NCORES = 8
TC = 8192 // NCORES
NHG = 8
NOB = 8 // NCORES
D = 4096
KC = D // 128
T = 8192
TOWN = 1024
NMOD = 6
A_W = 2048
B_W = 2048
HD = 64
NH = 32
DLORA = 96
GLORA = 256
B_COLS = 3 * B_W + 2 * DLORA + GLORA
IN_COLS = 2 * A_W + B_COLS
NE = 128
DE = 384
NORM_EPS = 1e-6
LN_EPS = 1e-5
GN_EPS = 64e-5
EXPM05 = float(np.exp(-0.5))

ENGS = ("pe", "act", "dve", "pool", "sp")


class Buf:
    __slots__ = ("name", "w", "r", "dsem", "dcnt")

    def __init__(self, name):
        self.name = name
        self.w = []
        self.r = []
        self.dsem = None
        self.dcnt = 0


class Sched:
    def __init__(self, nc, es):
        self.nc = nc
        self.es = es
        self.q = {e: [] for e in ENGS}
        self.cnt = {e: 0 for e in ENGS}
        self.sem = {e: es.enter_context(nc.semaphore("s_" + e)) for e in ENGS}
        self.waited = {e: {} for e in ENGS}
        self.semobj = {e: self.sem[e] for e in ENGS}
        self.dsems = {}
        self.ninst = 0

    def _dsem(self, b):
        rec = self.dsems.get(b.name)
        if rec is None:
            sem = self.es.enter_context(self.nc.semaphore("d%d_%s" % (len(self.dsems), b.name)))
            rec = {"sem": sem, "cnt": 0}
            self.dsems[b.name] = rec
            self.semobj["d:" + b.name] = sem
        return rec

    def _wait(self, eng, tok):
        kind, key, val = tok
        if kind == "eng" and key == eng and eng == "pe":
            return
        cur = self.waited[eng].get(key, 0)
        if cur >= val:
            return
        self.waited[eng][key] = val
        sem = self.semobj[key]
        self.q[eng].append(lambda e, sem=sem, val=val: e.wait_ge(sem, val))

    def _deps(self, eng, reads, writes):
        for b in reads:
            for tok in b.w:
                self._wait(eng, tok)
        for b in writes:
            for tok in b.w:
                self._wait(eng, tok)
            for tok in b.r:
                self._wait(eng, tok)

    def op(self, eng, fn, reads=(), writes=()):
        if getattr(self, "limit", None) and self.ninst >= self.limit:
            return
        self._deps(eng, reads, writes)
        self.cnt[eng] += 1
        n = self.cnt[eng]
        sem = self.sem[eng]
        self.q[eng].append(lambda e, fn=fn, sem=sem: fn(e).then_inc(sem, 1))
        tok = ("eng", eng, n)
        for b in reads:
            b.r.append(tok)
        for b in writes:
            b.w = [tok]
            b.r = []
        self.ninst += 1

    def dma(self, q, out, in_, reads=(), writes=(), owner=None, keep_w=False, **kw):
        if getattr(self, "limit", None) and self.ninst >= self.limit and not kw.pop("force", False):
            return
        kw.pop("force", None)
        if owner is None:
            owner = writes[0]
        rec = self._dsem(owner)
        sem = rec["sem"]
        okey = "d:" + owner.name
        for b in reads:
            for tok in b.w:
                self._wait(q, tok)
        for b in writes:
            if not keep_w:
                for tok in b.w:
                    self._wait(q, tok)
            for tok in b.r:
                self._wait(q, tok)
        rec["cnt"] += 16
        val = rec["cnt"]
        self.q[q].append(lambda e, out=out, in_=in_, sem=sem, kw=kw:
                         e.dma_start(out=out, in_=in_, **kw).then_inc(sem, 16))
        tok = ("dma", okey, val)
        for b in reads:
            b.r.append(tok)
        for b in writes:
            if keep_w:
                b.w = [t for t in b.w if not (t[0] == "dma" and t[1] == okey)] + [tok]
            else:
                b.w = [tok]
            b.r = []
        self.ninst += 1

    def barrier(self):
        for e in ENGS:
            for f in ENGS:
                if f != e and self.cnt[f] > 0:
                    self._wait(e, ("eng", f, self.cnt[f]))
            for name, rec in self.dsems.items():
                if rec["cnt"] > 0:
                    self._wait(e, ("dma", "d:" + name, rec["cnt"]))

    def final_wait(self, eng="sp"):
        for f in ENGS:
            if f != eng and self.cnt[f] > 0:
                self._wait(eng, ("eng", f, self.cnt[f]))
        for name, rec in self.dsems.items():
            if rec["cnt"] > 0:
                self._wait(eng, ("dma", "d:" + name, rec["cnt"]))

    def emit(self):
        nc = self.nc
        with nc.Block() as block:
            @block.tensor
            def _(e):
                for t in self.q["pe"]:
                    t(e)

            @block.scalar
            def _(e):
                for t in self.q["act"]:
                    t(e)

            @block.vector
            def _(e):
                for t in self.q["dve"]:
                    t(e)

            @block.gpsimd
            def _(e):
                for t in self.q["pool"]:
                    t(e)

            @block.sync
            def _(e):
                for t in self.q["sp"]:
                    t(e)


class Tl:
    n = 0

    def __init__(self, nc, stack, name, shape, dt, psum=False):
        f = nc.psum_tensor if psum else nc.sbuf_tensor
        Tl.n += 1
        self.t = stack.enter_context(f("t%d_%s" % (Tl.n, name), list(shape), dt))
        self.b = Buf(name)

    def __getitem__(self, k):
        return self.t[k]


def build_nc(nblk=None, stop=None, nhg=None, nob=None, limit=None, fake_mod=False):
    nc = bass.Bass("TRN2", target_bir_lowering=False)
    es = ExitStack()
    S = Sched(nc, es)
    S.limit = limit

    in_names = []
    order = ["M", "B", "A", "S", "O", "R", "E", None]

    def din(name, shape, dt=F32, need=None):
        if need is not None and order.index(stop) < order.index(need):
            return None
        in_names.append(name)
        return nc.dram_tensor(name, list(shape), dt, kind="ExternalInput").ap()

    x_in = din("x", [T, D])
    x_own = din("x_own", [TC, D], need="A")
    csel_d = din("csel", [128, NCORES], need="A")
    c_in = din("c", [KC, 128])
    mod_w = None if fake_mod else din("mod_w", [D, NMOD * D])
    mod_b = din("mod_b", [1, NMOD * D])
    n1g_d = din("n1g", [KC, 128])
    n2g_d = din("n2g", [KC, 128])
    fing_d = din("fing", [1, D])
    w_in_a = din("w_in_a", [D, 2 * A_W], need="A")
    w_in_b = din("w_in_b", [NHG, D, 1280])
    mu_d = din("mu", [NHG, 10, 128])
    w_out_d = din("w_out", [D, D], need="O")
    alng_d = din("alng", [1, A_W])
    alnb_d = din("alnb", [1, A_W])
    spw_d = din("spw", [16, 128, 128])
    spb_d = din("spb", [1, A_W])
    dup_d = din("dup", [NHG, DLORA, 256])
    iup_d = din("iup", [NHG, DLORA, 256])
    gup_d = din("gup", [NHG, GLORA, 256])
    vecs_d = din("vecs", [NHG, 16, 128])
    rw_d = din("rw", [D, NE])
    rb_d = din("rb", [1, NE])
    NEO = NE + 1
    ewg_d = din("ewg", [NEO, D, DE], need="E")
    ewu_d = din("ewu", [NEO, D, DE], need="E")
    ewd_d = din("ewd", [NEO, DE, D], need="E")
    ident_d = din("ident", [128, 128])
    bones_d = din("bones", [128, 128])
    mask4_d = din("mask4", [128, 512])
    maskl_d = din("maskl", [128, 256])
    i64s_d = din("i64s", [128, 64])
    out_d = nc.dram_tensor("out", [TC, D], F32, kind="ExternalOutput").ap()

    agin = nc.dram_tensor("agin", [24, 128], F32)
    agout = nc.dram_tensor("agout", [192, 128], F32)
    ybin = nc.dram_tensor("ybin", [256, T], BF16)
    ybout = nc.dram_tensor("ybout", [2048, T], BF16)
    x2_d = nc.dram_tensor("x2s", [TC, D], F32)
    b_agin, b_agout, b_ybin, b_ybout, b_x2 = (Buf(n) for n in ("agin", "agout", "ybin", "ybout", "x2s"))
    b_out = Buf("out")
    xin = nc.dram_tensor("xin", [TOWN, D], F32)
    xall = nc.dram_tensor("xall", [T, D], F32)
    wina_in = nc.dram_tensor("wina_in", [D // NCORES, 2 * A_W], F32)
    wina = nc.dram_tensor("wina", [D, 2 * A_W], F32)
    wout_in = nc.dram_tensor("wout_in", [D // NCORES, D], F32)
    wout = nc.dram_tensor("wout", [D, D], F32)
    x2all = nc.dram_tensor("x2all", [T, D], F32)
    part_d = nc.dram_tensor("part", [T, D], F32)
    psum_d = nc.dram_tensor("partsum", [T, D], F32)
    b_xin, b_xall, b_wina_in, b_wina, b_wout_in, b_wout, b_x2all, b_part, b_psum = (
        Buf(n) for n in ("xin", "xall", "wina_in", "wina", "wout_in", "wout", "x2all", "part", "partsum"))
    dbg = {}

    def dbg_out(name, shape, dt=F32):
        dbg[name] = nc.dram_tensor("dbg_" + name, list(shape), dt, kind="ExternalOutput").ap()
        return dbg[name]

    def finish():
        S.final_wait("sp")
        S.emit()
        nc._in_names = in_names
        nc._dbg = sorted(dbg)
        print("instr per engine", S.cnt, "dma sems", len(S.dsems))
        return nc
    ccs = es.enter_context(nc.semaphore("ccs"))
    S.semobj["ccs"] = ccs
    cc_count = [0]

    def allgather(src, dst, bsrc, bdst, kind="AllGather"):
        S._deps("pool", [bsrc], [bdst])
        cc_count[0] += 1
        n = cc_count[0]
        op = ALU.add if kind == "AllReduce" else ALU.bypass
        S.q["pool"].append(lambda e: e.collective_compute(
            kind, op, replica_groups=[list(range(NCORES))],
            ins=[src.ap().opt()], outs=[dst.ap().opt()]).then_inc(ccs, 1))
        tok = ("cc", "ccs", n)
        bsrc.r.append(tok)
        bdst.w = [tok]
        bdst.r = []

    def ACT(out, in_, func, R, W, **kw):
        S.op("act", lambda e: e.activation(out=out, in_=in_, func=func, **kw), R, W)

    def TT(eng, out, a, b, op, R, W):
        S.op(eng, lambda e: e.tensor_tensor(out=out, in0=a, in1=b, op=op), R, W)

    def TS(eng, out, a, s1, s2, op0, op1, R, W):
        if op1 is None:
            S.op(eng, lambda e: e.tensor_scalar(out=out, in0=a, scalar1=s1, scalar2=None, op0=op0), R, W)
        else:
            S.op(eng, lambda e: e.tensor_scalar(out=out, in0=a, scalar1=s1, scalar2=s2, op0=op0, op1=op1), R, W)

    def STT(out, a, s, b, op0, op1, R, W):
        S.op("dve", lambda e: e.scalar_tensor_tensor(out=out, in0=a, scalar=s, in1=b, op0=op0, op1=op1), R, W)

    def MM(out, lhsT, rhs, start, stop, R, W):
        S.op("pe", lambda e: e.matmul(out, lhsT, rhs, start=start, stop=stop), R, W)

    def CP(eng, out, in_, R, W):
        if eng == "act":
            S.op("act", lambda e: e.copy(out=out, in_=in_), R, W)
        else:
            S.op(eng, lambda e: e.tensor_copy(out=out, in_=in_), R, W)

    def RECIP(out, in_, R, W):
        S.op("dve", lambda e: e.reciprocal(out=out, in_=in_), R, W)

    def MEMSET(eng, out, val, W):
        S.op(eng, lambda e: e.memset(out, val), [], W)

    P = ExitStack()
    es.enter_context(P)

    def tile(name, shape, dt=F32, stack=None):
        return Tl(nc, stack or P, name, shape, dt)

    PS = [Tl(nc, P, "ps%d" % i, [128, 512], F32, psum=True) for i in range(8)]
    psi = [0]

    def ps():
        psi[0] = (psi[0] + 1) % 8
        return PS[psi[0]]

    ident = tile("ident", [128, 128])
    bones = tile("bones", [128, 128])
    mask4 = tile("mask4", [128, 512])
    maskl = tile("maskl", [128, 256])
    i64s = tile("i64s", [128, 64])
    i2 = tile("i2", [128, 256])
    ones = tile("ones", [128, 128])
    S.dma("sp", ident[:], ident_d, writes=[ident.b])
    S.dma("sp", bones[:], bones_d, writes=[bones.b])
    S.dma("sp", mask4[:], mask4_d, writes=[mask4.b])
    S.dma("sp", maskl[:], maskl_d, writes=[maskl.b])
    S.dma("sp", i64s[:], i64s_d, writes=[i64s.b])
    S.dma("sp", i2[:, 0:128], ident_d, writes=[i2.b])
    S.dma("sp", i2[:, 128:256], ident_d, writes=[i2.b], keep_w=True)
    MEMSET("pool", ones[:], 1.0, [ones.b])

    def TR(out, in_, k, R, W):
        S.op("pe", lambda e: e.transpose(out, in_, ident[0:k, 0:k]), list(R) + [ident.b], W)

    def load_pl(name, dram_rows, k, stack=None):
        o = tile(name, [128, k], F32, stack)
        with ExitStack() as tmp:
            rows = Tl(nc, tmp, name + "_rows", [k, 128], F32)
            S.dma("sp", rows[:], dram_rows, writes=[rows.b])
            p = ps()
            TR(p[:, 0:k], rows[:], k, [rows.b], [p.b])
            CP("dve", o[:], p[:, 0:k], [p.b], [o.b])
            S.barrier()
        return o

    cT = load_pl("cT", c_in, KC)
    ACT(cT[:], cT[:], AF.Silu, [cT.b], [cT.b])
    n1g = load_pl("n1gT", n1g_d, KC)
    n2g = load_pl("n2gT", n2g_d, KC)
    modT = tile("modT", [128, 192])
    with ExitStack() as ph:
        mw = [Tl(nc, ph, "mw%d" % i, [128, 3072], F32) for i in range(3)]
        mrow = Tl(nc, ph, "mrow", [1, 3072], F32)
        mbrow = Tl(nc, ph, "mbrow", [1, 3072], F32)
        mrows = [Tl(nc, ph, "mrows%d" % i, [96, 128], F32) for i in range(2)]
        if fake_mod:
            MEMSET("pool", mrow[:], 0.1, [mrow.b])
            for gq in range(8):
                S.dma("sp", agout.ap()[gq * 24:(gq + 1) * 24, :].rearrange("(o a) b -> o (a b)", o=1), mrow[:],
                      reads=[mrow.b], writes=[b_agout], keep_w=True)
        for gq in range(0 if fake_mod else 8):
            gsl = slice(gq * 3072, (gq + 1) * 3072)
            S.dma("sp", mbrow[:], mod_b[:, gsl], writes=[mbrow.b])
            for k in range(KC):
                w = mw[k % 3]
                S.dma("sp", w[:], mod_w[k * 128:(k + 1) * 128, gsl], writes=[w.b])
                for b in range(6):
                    MM(PS[b][0:1, :], cT[:, k:k + 1], w[:, b * 512:(b + 1) * 512], k == 0, k == KC - 1,
                       [cT.b, w.b], [PS[b].b])
            for b in range(6):
                TT("dve", mrow[:, b * 512:(b + 1) * 512], PS[b][0:1, :], mbrow[:, b * 512:(b + 1) * 512], ALU.add,
                   [PS[b].b, mbrow.b], [mrow.b])
            S.dma("sp", agout.ap()[gq * 24:(gq + 1) * 24, :].rearrange("(o a) b -> o (a b)", o=1), mrow[:],
                  reads=[mrow.b], writes=[b_agout], keep_w=True)
        for i in range(2):
            S.dma("sp", mrows[i][:], agout.ap()[i * 96:(i + 1) * 96, :], reads=[b_agout], writes=[mrows[i].b])
            p = ps()
            TR(p[:, 0:96], mrows[i][:], 96, [mrows[i].b], [p.b])
            CP("dve", modT[:, i * 96:(i + 1) * 96], p[:, 0:96], [p.b], [modT.b])
        S.barrier()
    s1 = tile("s1", [128, KC])
    s2 = tile("s2", [128, KC])
    STT(s1[:], modT[:, 32:64], 1.0, n1g[:], ALU.add, ALU.mult, [modT.b, n1g.b], [s1.b])
    STT(s2[:], modT[:, 128:160], 1.0, n2g[:], ALU.add, ALU.mult, [modT.b, n2g.b], [s2.b])
    sh1 = modT[:, 0:32]
    sh2 = modT[:, 96:128]
    if stop == "M":
        S.dma("sp", dbg_out("modT", [128, 192]), modT[:], reads=[modT.b], writes=[Buf("dbgm")])
        return finish()

    def bc_row(dst, src_row_ap, R, W):
        S.dma("sp", dst, src_row_ap.partition_broadcast(128).squeeze(1), reads=R, writes=W)

    junk = tile("junk", [128, D], BF16)
    ssq = tile("ssq", [128, 1])
    rstd = tile("rstd", [128, 1])

    def norm_T(xt, sc, sh, shb, dsts):
        ACT(junk[:], xt[:], AF.Square, [xt.b], [junk.b, ssq.b], accum_out=ssq[:])
        TS("dve", rstd[:], ssq[:], 1.0 / D, NORM_EPS, ALU.mult, ALU.add, [ssq.b], [rstd.b])
        ACT(rstd[:], rstd[:], AF.Sqrt, [rstd.b], [rstd.b])
        RECIP(rstd[:], rstd[:], [rstd.b], [rstd.b])
        TS("dve", xt[:], xt[:], rstd[:, 0:1], None, ALU.mult, None, [xt.b, rstd.b], [xt.b])
        for c4 in range(KC // 4):
            p = ps()
            for j in range(4):
                c = c4 * 4 + j
                TR(p[:, j * 128:(j + 1) * 128], xt[:, c * 128:(c + 1) * 128], 128, [xt.b], [p.b])
            for j in range(4):
                c = c4 * 4 + j
                for fn, b in dsts:
                    ACT(fn(c), p[:, j * 128:(j + 1) * 128], AF.Identity, [p.b, sc.b, shb], [b],
                        scale=sc[:, c:c + 1], bias=sh[:, c:c + 1])

    def load_w_bf16(dst_fn, src_rows_fn, nchunks, ncols, stg, R_extra=()):
        for c in range(nchunks):
            st = stg[c % len(stg)]
            S.dma("sp", st[:, 0:ncols], src_rows_fn(c), reads=list(R_extra), writes=[st.b])
            ap, b = dst_fn(c)
            CP("pool", ap, st[:, 0:ncols], [st.b], [b])

    NHG_RUN = nhg or NHG
    BLK = 256
    NBLK = nblk or (T // BLK)
    h1s = nc.dram_tensor("h1s", [NBLK, 128, KC * BLK], BF16)
    b_h1s = Buf("h1s")
    with ExitStack() as pre:
        xpre = Tl(nc, pre, "xpre", [128, D], F32)
        hpre = [Tl(nc, pre, "hpre%d" % i, [128, KC, BLK], BF16) for i in range(2)]
        for blk in range(NBLK):
            hp_ = hpre[blk % 2]
            for tt in range(BLK // 128):
                r0 = blk * BLK + tt * 128
                S.dma("sp", xpre[:], x_in[r0:r0 + 128, :], writes=[xpre.b])
                norm_T(xpre, s1, sh1, modT.b,
                       [(lambda c, tt=tt, hp_=hp_: hp_[:, c, tt * 128:(tt + 1) * 128], hp_.b)])
            S.dma("act", h1s.ap()[blk], hp_[:].rearrange("p k t -> p (k t)"), reads=[hp_.b], writes=[b_h1s], keep_w=True)
        S.barrier()
    for hg in range(NHG_RUN):
        C0 = EXPM05
        BLK = 256
        NBLK = nblk or (T // BLK)
        with ExitStack() as ph:
            def pt(name, shape, dt=F32):
                return Tl(nc, ph, name, shape, dt)
            vecs = load_pl("vecsT", vecs_d[hg], 16, ph)
            muT = load_pl("muT", mu_d[hg], 10, ph)
            omka = pt("omka", [128, 2])
            TS("dve", omka[:], vecs[:, 6:8], -1.0, 1.0, ALU.mult, ALU.add, [vecs.b], [omka.b])
            WB = pt("WB", [128, KC, 1280], BF16)
            dupw = pt("dupw", [DLORA, 256], BF16)
            iupw = pt("iupw", [DLORA, 256], BF16)
            gupw = pt("gupw", [128, 2, 256], BF16)
            with ExitStack() as tmp:
                stg = [Tl(nc, tmp, "stgB%d" % i, [128, 1280], F32) for i in range(2)]
                load_w_bf16(lambda c: (WB[:, c, :], WB.b), lambda c: w_in_b[hg, c * 128:(c + 1) * 128, :], KC, 1280, stg)
                S.dma("sp", stg[0][0:DLORA, 0:256], dup_d[hg], writes=[stg[0].b])
                CP("pool", dupw[:], stg[0][0:DLORA, 0:256], [stg[0].b], [dupw.b])
                S.dma("sp", stg[1][0:DLORA, 0:256], iup_d[hg], writes=[stg[1].b])
                CP("pool", iupw[:], stg[1][0:DLORA, 0:256], [stg[1].b], [iupw.b])
                for j in range(2):
                    S.dma("sp", stg[j][:, 0:256], gup_d[hg, j * 128:(j + 1) * 128, :], writes=[stg[j].b])
                    CP("pool", gupw[:, j, :], stg[j][:, 0:256], [stg[j].b], [gupw.b])
                S.barrier()
            xts = [pt("xtB0", [128, D])] * 2
            h1T = pt("h1T", [128, KC, BLK], BF16)
            pB = [pt("pB%d" % i, [128, BLK + 1]) for i in range(10)]
            pS = [pt("pS%d" % i, [128, BLK]) for i in range(10)]
            tmpd = pt("tmpd", [128, BLK])
            twd = pt("twd", [128, BLK], BF16)
            adb = pt("adb", [128, BLK], BF16)
            sgd = pt("sgd", [128, 2, BLK], BF16)
            sg = pt("sg", [128, BLK]); av = pt("av", [128, BLK]); gate = [pt("gate%d" % i, [128, BLK]) for i in range(2)]
            sq = pt("sq", [128, BLK]); rinv = pt("rinv", [128, BLK]); kk = pt("kk", [128, BLK])
            tk = pt("tk", [128, BLK]); kmod = pt("kmod", [128, BLK]); bvec = pt("bvec", [128, BLK])
            rkb = pt("rkb", [128, BLK]); bterm = [pt("bterm%d" % i, [128, BLK]) for i in range(2)]
            Gc = pt("Gc", [128, BLK]); Gx = pt("Gx", [128, BLK]); nb = pt("nb", [128, 2]); PCt = pt("PCt", [128, 2])
            E1 = pt("E1", [128, BLK]); Em = pt("Em", [128, BLK]); Ep = pt("Ep", [128, BLK]); Ee = pt("Ee", [128, BLK])
            BKt = [pt("BKt%d" % i, [128, 2, BLK], BF16) for i in range(2)]
            ARt = [pt("ARt%d" % i, [128, 2, BLK], BF16) for i in range(2)]
            TM = [pt("TM%d" % i, [128, 4, BLK]) for i in range(2)]
            TOK = pt("TOK", [128, 4, 128], BF16)
            ABm = pt("ABm", [128, 512], BF16); AKm = pt("AKm", [128, 512], BF16)
            Lk = [pt("Lk%d" % i, [128, 256], BF16) for i in range(2)]
            Mk = [pt("Mk%d" % i, [128, 256], BF16) for i in range(2)]
            Tt = [pt("Tt%d" % i, [128, 256], BF16) for i in range(2)]
            Xs = pt("Xs", [128, 128], BF16)
            WU = pt("WU", [128, 256], BF16)
            QT = pt("QT", [128, 128], BF16)
            Mc = pt("Mc", [128, 64], BF16)
            NcT = pt("NcT", [128, 64])
            ST = [[pt("ST%d_%d" % (ct, i), [128, 64], BF16) for i in range(2)] for ct in range(2)]
            st6 = pt("st6", [128, 2, 6]); mv = pt("mv", [128, 2, 2]); grs = pt("grs", [128, 2])
            yn = pt("yn", [128, 128]); ybn = pt("ybn", [128, 128])
            ybo = [pt("ybo%d" % i, [128, BLK], BF16) for i in range(2)]
            for i in range(10):
                MEMSET("pool", pB[i][:, 0:1], 0.0, [pB[i].b])
            for ct in range(2):
                MEMSET("pool", ST[ct][0][:], 0.0, [ST[ct][0].b])
            stp = [0, 0]

            for blk in range(NBLK):
                S.dma("sp", h1T[:].rearrange("p k t -> p (k t)"), h1s.ap()[blk], reads=[b_h1s], writes=[h1T.b])
                for ci in range(10):
                    p = ps()
                    for k in range(KC):
                        MM(p[:, 0:BLK], WB[:, k, ci * 128:(ci + 1) * 128], h1T[:, k, :], k == 0, k == KC - 1,
                           [WB.b, h1T.b], [p.b])
                    CP("act", pB[ci][:, 1:BLK + 1], p[:, 0:BLK], [p.b], [pB[ci].b])
                    TT("dve", tmpd[:], pB[ci][:, 0:BLK], pB[ci][:, 1:BLK + 1], ALU.subtract, [pB[ci].b], [tmpd.b])
                    STT(pS[ci][:], tmpd[:], muT[:, ci:ci + 1], pB[ci][:, 1:BLK + 1], ALU.mult, ALU.add,
                        [tmpd.b, muT.b, pB[ci].b], [pS[ci].b])
                    CP("pool", pB[ci][:, 0:1], pB[ci][:, BLK:BLK + 1], [pB[ci].b], [pB[ci].b])
                ACT(twd[0:DLORA, :], pS[6][0:DLORA, :], AF.Tanh, [pS[6].b], [twd.b])
                CP("pool", adb[0:DLORA, :], pS[7][0:DLORA, :], [pS[7].b], [adb.b])
                for j in range(2):
                    ACT(sgd[:, j, :], pS[8 + j][:], AF.Sigmoid, [pS[8 + j].b], [sgd.b])
                for ct in range(2):
                    cs = slice(ct * 128, (ct + 1) * 128)
                    r_, k_, v_ = pS[ct], pS[2 + ct], pS[4 + ct]
                    p = ps()
                    MM(p[:, 0:BLK], dupw[:, cs], twd[0:DLORA, :], True, True, [dupw.b, twd.b], [p.b])
                    ACT(sg[:], p[:, 0:BLK], AF.Sigmoid, [p.b, vecs.b], [sg.b], bias=vecs[:, 0 + ct:1 + ct])
                    p = ps()
                    MM(p[:, 0:BLK], iupw[:, cs], adb[0:DLORA, :], True, True, [iupw.b, adb.b], [p.b])
                    ACT(av[:], p[:, 0:BLK], AF.Sigmoid, [p.b, vecs.b], [av.b], bias=vecs[:, 2 + ct:3 + ct])
                    p = ps()
                    for j in range(2):
                        MM(p[:, 0:BLK], gupw[:, j, cs], sgd[:, j, :], j == 0, j == 1, [gupw.b, sgd.b], [p.b])
                    CP("act", gate[ct][:], p[:, 0:BLK], [p.b], [gate[ct].b])
                    ACT(sq[:], k_[:], AF.Square, [k_.b, vecs.b], [sq.b], scale=vecs[:, 4 + ct:5 + ct])
                    p = ps()
                    MM(p[:, 0:BLK], bones[:], sq[:], True, True, [bones.b, sq.b], [p.b])
                    ACT(rinv[:], p[:, 0:BLK], AF.Sqrt, [p.b], [rinv.b])
                    TS("dve", rinv[:], rinv[:], 1e-12, None, ALU.max, None, [rinv.b], [rinv.b])
                    RECIP(rinv[:], rinv[:], [rinv.b], [rinv.b])
                    STT(kk[:], k_[:], vecs[:, 4 + ct:5 + ct], rinv[:], ALU.mult, ALU.mult, [k_.b, vecs.b, rinv.b], [kk.b])
                    TS("dve", tk[:], av[:], vecs[:, 6 + ct:7 + ct], omka[:, ct:ct + 1], ALU.mult, ALU.add,
                       [av.b, vecs.b, omka.b], [tk.b])
                    TT("pool", kmod[:], k_[:], tk[:], ALU.mult, [k_.b, tk.b], [kmod.b])
                    TT("pool", bvec[:], kk[:], av[:], ALU.mult, [kk.b, av.b], [bvec.b])
                    STT(rkb[:], r_[:], vecs[:, 8 + ct:9 + ct], kmod[:], ALU.mult, ALU.mult, [r_.b, vecs.b, kmod.b], [rkb.b])
                    p = ps()
                    MM(p[:, 0:BLK], bones[:], rkb[:], True, True, [bones.b, rkb.b], [p.b])
                    TT("dve", bterm[ct][:], p[:, 0:BLK], v_[:], ALU.mult, [p.b, v_.b], [bterm[ct].b])
                    for u in range(2):
                        us = slice(u * 128, (u + 1) * 128)
                        S.op("dve", lambda e, us=us: e.tensor_tensor_scan(out=Gc[:, us], data0=ones[:, 0:128], data1=sg[:, us],
                                                                        initial=0.0, op0=ALU.mult, op1=ALU.add),
                             [ones.b, sg.b], [Gc.b])
                        TS("dve", nb[:, u:u + 1], Gc[:, u * 128 + 127:u * 128 + 128], -C0, None, ALU.mult, None, [Gc.b], [nb.b])
                    TT("pool", Gx[:], Gc[:], sg[:], ALU.subtract, [Gc.b, sg.b], [Gx.b])
                    ACT(E1[:], Gc[:], AF.Exp, [Gc.b], [E1.b], scale=-C0)
                    ACT(Em[:], Gc[:], AF.Exp, [Gc.b], [Em.b], scale=C0)
                    ACT(Ep[:], Gx[:], AF.Exp, [Gx.b], [Ep.b], scale=-C0)
                    for u in range(2):
                        us = slice(u * 128, (u + 1) * 128)
                        ACT(Ee[:, us], Gc[:, us], AF.Exp, [Gc.b, nb.b], [Ee.b], scale=C0, bias=nb[:, u:u + 1])
                    ACT(PCt[:], nb[:], AF.Exp, [nb.b], [PCt.b])
                    bk, ar, tm = BKt[ct], ARt[ct], TM[ct]
                    TT("dve", bk[:, 0, :], bvec[:], Em[:], ALU.mult, [bvec.b, Em.b], [bk.b])
                    TT("dve", bk[:, 1, :], kmod[:], Em[:], ALU.mult, [kmod.b, Em.b], [bk.b])
                    STT(tm[:, 0, :], kk[:], -1.0, Ep[:], ALU.mult, ALU.mult, [kk.b, Ep.b], [tm.b])
                    CP("pool", ar[:, 0, :], tm[:, 0, :], [tm.b], [ar.b])
                    TT("dve", ar[:, 1, :], r_[:], E1[:], ALU.mult, [r_.b, E1.b], [ar.b])
                    CP("pool", tm[:, 1, :], v_[:], [v_.b], [tm.b])
                    TT("pool", tm[:, 2, :], bvec[:], Ee[:], ALU.mult, [bvec.b, Ee.b], [tm.b])
                    TT("pool", tm[:, 3, :], kmod[:], Ee[:], ALU.mult, [kmod.b, Ee.b], [tm.b])

                    for u in range(2):
                        us = slice(u * 128, (u + 1) * 128)
                        p = ps()
                        for j in range(4):
                            TR(p[:, j * 128:(j + 1) * 128], tm[:, j, us], 128, [tm.b], [p.b])
                        CP("act", TOK[:].rearrange("p a b -> p (a b)"), p[:, :], [p.b], [TOK.b])
                        for h in range(2):
                            hp = slice(h * 64, (h + 1) * 64)
                            pa = ps()
                            MM(pa[:, 0:256], bk[hp, 0, us], ar[hp, :, us], True, True, [bk.b, ar.b], [pa.b])
                            MM(pa[:, 256:512], bk[hp, 1, us], ar[hp, :, us], True, True, [bk.b, ar.b], [pa.b])
                            pc = ps()
                            MM(pc[:, 0:128], ar[hp, 0, us], bk[hp, 0, us], True, True, [bk.b, ar.b], [pc.b])
                            TT("dve", ABm[:, h * 256:(h + 1) * 256], pa[:, 0:256], mask4[:, 0:256], ALU.mult, [pa.b, mask4.b], [ABm.b])
                            TT("dve", AKm[:, h * 256:(h + 1) * 256], pa[:, 256:512], mask4[:, 0:256], ALU.mult, [pa.b, mask4.b], [AKm.b])
                            TT("dve", Lk[0][:, h * 128:(h + 1) * 128], pc[:, 0:128], maskl[:, 0:128], ALU.mult, [pc.b, maskl.b], [Lk[0].b])
                        AB3 = ABm[:].rearrange("p (h x) -> p h x", h=2)
                        AK3 = AKm[:].rearrange("p (h x) -> p h x", h=2)
                        CP("pool", Mk[0][:].rearrange("p (h x) -> p h x", h=2), AB3[:, :, 0:128], [ABm.b], [Mk[0].b])
                        TT("pool", Tt[0][:], Mk[0][:], i2[:], ALU.add, [Mk[0].b, i2.b], [Tt[0].b])
                        px = ps()
                        for h in range(2):
                            MM(px[:, h * 64:(h + 1) * 64], AK3[:, h, 0:128], TOK[:, 1, h * 64:(h + 1) * 64], True, True,
                               [AKm.b, TOK.b], [px.b])
                        CP("act", Xs[:], px[:, 0:128], [px.b], [Xs.b])
                        ci_, ti_ = 0, 0
                        for it in range(6):
                            mk, lk, tcur = Mk[ci_], Lk[ci_], Tt[ti_]
                            mk2, lk2, tnew = Mk[1 - ci_], Lk[1 - ci_], Tt[1 - ti_]
                            if it < 5:
                                pm = ps()
                                for h in range(2):
                                    hs = slice(h * 128, (h + 1) * 128)
                                    MM(pm[:, hs], lk[:, hs], mk[:, hs], True, True, [lk.b, mk.b], [pm.b])
                            pl = ps()
                            for h in range(2):
                                hs = slice(h * 128, (h + 1) * 128)
                                MM(pl[:, hs], mk[:, hs], lk[:, hs], True, True, [lk.b, mk.b], [pl.b])
                            if it < 5:
                                CP("act", mk2[:], pm[:, 0:256], [pm.b], [mk2.b])
                            CP("dve", lk2[:], pl[:, 0:256], [pl.b], [lk2.b])
                            pt_ = ps()
                            for h in range(2):
                                hs = slice(h * 128, (h + 1) * 128)
                                MM(pt_[:, hs], lk2[:, hs], tcur[:, hs], True, True, [lk2.b, tcur.b], [pt_.b])
                            TT("dve", tnew[:], pt_[:, 0:256], tcur[:], ALU.add, [pt_.b, tcur.b], [tnew.b])
                            ci_, ti_ = 1 - ci_, 1 - ti_
                        tfin = Tt[ti_]
                        pw = ps()
                        for h in range(2):
                            hs = slice(h * 128, (h + 1) * 128)
                            MM(pw[:, h * 128:h * 128 + 64], tfin[:, hs], TOK[:, 0, h * 64:(h + 1) * 64], True, True,
                               [tfin.b, TOK.b], [pw.b])
                            MM(pw[:, h * 128 + 64:h * 128 + 128], tfin[:, hs], Xs[:, h * 64:(h + 1) * 64], True, True,
                               [tfin.b, Xs.b], [pw.b])
                        CP("act", WU[:], pw[:, 0:256], [pw.b], [WU.b])
                        pq = ps()
                        for h in range(2):
                            hp = slice(h * 64, (h + 1) * 64)
                            MM(pq[hp, 0:128], WU[:, h * 128:h * 128 + 64], AB3[:, h, 128:256], True, True, [WU.b, ABm.b], [pq.b])
                            MM(pq[hp, 128:192], WU[:, h * 128:h * 128 + 64], TOK[:, 2, h * 64:(h + 1) * 64], True, True,
                               [WU.b, TOK.b], [pq.b])
                        TT("dve", QT[:], pq[:, 0:128], ar[:, 1, us], ALU.add, [pq.b, ar.b], [QT.b])
                        STT(Mc[:], i64s[:], PCt[:, u:u + 1], pq[:, 128:192], ALU.mult, ALU.add, [i64s.b, PCt.b, pq.b], [Mc.b])
                        pn = ps()
                        for h in range(2):
                            hp = slice(h * 64, (h + 1) * 64)
                            MM(pn[hp, 0:64], TOK[:, 2, h * 64:(h + 1) * 64], WU[:, h * 128 + 64:h * 128 + 128], True, False,
                               [TOK.b, WU.b], [pn.b])
                            MM(pn[hp, 0:64], TOK[:, 3, h * 64:(h + 1) * 64], TOK[:, 1, h * 64:(h + 1) * 64], False, True,
                               [TOK.b], [pn.b])
                        CP("act", NcT[:], pn[:, 0:64], [pn.b], [NcT.b])
                        sold, snew = ST[ct][stp[ct]], ST[ct][1 - stp[ct]]
                        pys = [ps(), ps()]
                        for h in range(2):
                            hp = slice(h * 64, (h + 1) * 64)
                            py = pys[h]
                            yo = py[:, 0:64]
                            MM(yo, AB3[:, h, 128:256], WU[:, h * 128 + 64:h * 128 + 128], True, False, [ABm.b, WU.b], [py.b])
                            MM(yo, AK3[:, h, 128:256], TOK[:, 1, h * 64:(h + 1) * 64], False, False, [AKm.b, TOK.b], [py.b])
                            MM(yo, QT[hp, :], sold[hp, :], False, True, [QT.b, sold.b], [py.b])
                        for h in range(2):
                            hp = slice(h * 64, (h + 1) * 64)
                            pst = ps()
                            MM(pst[hp, 0:64], Mc[hp, :], sold[hp, :], True, True, [Mc.b, sold.b], [pst.b])
                            TT("dve", snew[hp, :], pst[hp, 0:64], NcT[hp, :], ALU.add, [pst.b, NcT.b, snew.b], [snew.b])
                        stp[ct] = 1 - stp[ct]
                        for h in range(2):
                            S.op("dve", lambda e, h=h, py=pys[h]: e.bn_stats(out=st6[:, h, :], in_=py[:, 0:64]), [pys[h].b], [st6.b])
                            S.op("dve", lambda e, h=h: e.bn_aggr(out=mv[:, h, :], in_=st6[:, h, :]), [st6.b], [mv.b])
                        TS("dve", grs[:], mv[:, :, 1], GN_EPS, None, ALU.add, None, [mv.b], [grs.b])
                        ACT(grs[:], grs[:], AF.Sqrt, [grs.b], [grs.b])
                        RECIP(grs[:], grs[:], [grs.b], [grs.b])
                        for h in range(2):
                            TS("dve", yn[:, h * 64:(h + 1) * 64], pys[h][:, 0:64], mv[:, h, 0:1], grs[:, h:h + 1],
                               ALU.subtract, ALU.mult, [pys[h].b, mv.b, grs.b, yn.b], [yn.b])
                        pz = ps()
                        TR(pz[:, 0:128], yn[:], 128, [yn.b], [pz.b])
                        ACT(ybn[:], pz[:, 0:128], AF.Identity, [pz.b, vecs.b], [ybn.b],
                            scale=vecs[:, 10 + ct:11 + ct], bias=vecs[:, 12 + ct:13 + ct])
                        TT("pool", ybn[:], ybn[:], bterm[ct][:, us], ALU.add, [ybn.b, bterm[ct].b], [ybn.b])
                        TT("pool", ybo[ct][:, us], ybn[:], gate[ct][:, us], ALU.mult, [ybn.b, gate[ct].b], [ybo[ct].b])
                    S.dma("act", ybout.ap()[hg * 256 + ct * 128:hg * 256 + (ct + 1) * 128, blk * BLK:(blk + 1) * BLK], ybo[ct][:],
                          reads=[ybo[ct].b], writes=[b_ybout], keep_w=True)
            S.barrier()

    if stop == "B":
        dy = dbg_out("yb", [NHG_RUN * 256, (nblk or 32) * 256], BF16)
        S.dma("sp", dy, ybout.ap()[0:NHG_RUN * 256, 0:(nblk or 32) * 256], reads=[b_ybout], writes=[Buf("dbgy")], force=True)
        return finish()

    NOB_RUN = nob or NOB
    for ob in range(NOB_RUN):
        tok0 = ob * TOWN
        MID = ExitStack()
        yaT = tile("yaT", [128, 16, TOWN], BF16, MID)
        HALF = 512
        with ExitStack() as ph:
            def pt(name, shape, dt=F32):
                return Tl(nc, ph, name, shape, dt)
            h1o = pt("h1o", [128, KC, HALF], BF16)
            vn = pt("vn", [128, 4, A_W], BF16)
            wsT = pt("wsT", [128, 16, 128], BF16)
            lng = pt("lng", [128, A_W]); lnb = pt("lnb", [128, A_W]); spb = pt("spb", [128, A_W])
            bc_row(lng[:], alng_d, [], [lng.b]); bc_row(lnb[:], alnb_d, [], [lnb.b]); bc_row(spb[:], spb_d, [], [spb.b])
            Wv = pt("Wv", [128, KC, 512], BF16)
            Wu = pt("Wu", [128, KC, 128], BF16)
            stg = [pt("stgA%d" % i, [128, 512]) for i in range(2)]
            vf = pt("vf", [128, 512]); vt = pt("vt", [128, 512])
            ast6 = pt("ast6", [128, 4, 6]); amv = pt("amv", [128, 4, 2]); ars = pt("ars", [128, 4])
            uT = pt("uT", [128, HALF]); mtmp = pt("mtmp", [128, 128])
            xts = [pt("xtA0", [128, D])] * 2
            for g in range(16):
                S.dma("sp", stg[g % 2][:, 0:128], spw_d[g], writes=[stg[g % 2].b])
                p = ps()
                TR(p[:, 0:128], stg[g % 2][:, 0:128], 128, [stg[g % 2].b], [p.b])
                TT("dve", wsT[:, g, :], p[:, 0:128], mask4[:, 128:256], ALU.mult, [p.b, mask4.b], [wsT.b])
            for hf in range(2):
                for tt in range(4):
                    xt = xts[tt % 2]
                    r0 = hf * HALF + tt * 128
                    S.dma("sp", xt[:], x_own[tok0 + r0:tok0 + r0 + 128, :], writes=[xt.b])
                    norm_T(xt, s1, sh1, modT.b, [(lambda c, tt=tt: h1o[:, c, tt * 128:(tt + 1) * 128], h1o.b)])
                for cb in range(4):
                    load_w_bf16(lambda c: (Wv[:, c, :], Wv.b),
                                lambda c, cb=cb: w_in_a[c * 128:(c + 1) * 128, A_W + cb * 512:A_W + (cb + 1) * 512], KC, 512, stg)
                    for tt in range(4):
                        p = ps()
                        for k in range(KC):
                            MM(p[:, :], h1o[:, k, tt * 128:(tt + 1) * 128], Wv[:, k, :], k == 0, k == KC - 1, [h1o.b, Wv.b], [p.b])
                        ACT(vf[:], p[:, :], AF.Gelu, [p.b], [vf.b])
                        for g in range(4):
                            gs = slice(g * 128, (g + 1) * 128)
                            S.op("dve", lambda e, g=g, gs=gs: e.bn_stats(out=ast6[:, g, :], in_=vf[:, gs]), [vf.b], [ast6.b])
                            S.op("dve", lambda e, g=g: e.bn_aggr(out=amv[:, g, :], in_=ast6[:, g, :]), [ast6.b], [amv.b])
                        TS("dve", ars[:], amv[:, :, 1], LN_EPS, None, ALU.add, None, [amv.b], [ars.b])
                        ACT(ars[:], ars[:], AF.Sqrt, [ars.b], [ars.b])
                        RECIP(ars[:], ars[:], [ars.b], [ars.b])
                        for g in range(4):
                            gs = slice(g * 128, (g + 1) * 128)
                            TS("dve", vt[:, gs], vf[:, gs], amv[:, g, 0:1], ars[:, g:g + 1], ALU.subtract, ALU.mult,
                               [vf.b, amv.b, ars.b], [vt.b])
                        cs = slice(cb * 512, (cb + 1) * 512)
                        TT("pool", vt[:], vt[:], lng[:, cs], ALU.mult, [vt.b, lng.b], [vt.b])
                        TT("pool", vn[:, tt, cs], vt[:], lnb[:, cs], ALU.add, [vt.b, lnb.b], [vn.b])
                for g in range(16):
                    load_w_bf16(lambda c: (Wu[:, c, :], Wu.b),
                                lambda c, g=g: w_in_a[c * 128:(c + 1) * 128, g * 128:(g + 1) * 128], KC, 128, stg)
                    p = ps()
                    for k in range(KC):
                        MM(p[:, :], Wu[:, k, :], h1o[:, k, :], k == 0, k == KC - 1, [Wu.b, h1o.b], [p.b])
                    ACT(uT[:], p[:, :], AF.Gelu, [p.b], [uT.b])
                    for tt in range(4):
                        p = ps()
                        MM(p[:, 0:128], vn[:, tt, g * 128:(g + 1) * 128], wsT[:, g, :], True, True, [vn.b, wsT.b], [p.b])
                        TT("dve", mtmp[:], p[:, 0:128], spb[:, g * 128:(g + 1) * 128], ALU.add, [p.b, spb.b], [mtmp.b])
                        c0 = hf * HALF + tt * 128
                        TT("pool", yaT[:, g, c0:c0 + 128], mtmp[:], uT[:, tt * 128:(tt + 1) * 128], ALU.mult,
                           [mtmp.b, uT.b], [yaT.b])
            S.barrier()

        ybT = tile("ybT", [128, 16, TOWN], BF16, MID)
        csel = tile("csel", [128, NCORES], F32, MID)
        S.dma("sp", csel[:], csel_d, writes=[csel.b])
        with ExitStack() as ysl:
            ycand = [Tl(nc, ysl, "ycand%d" % i, [128, TOWN], BF16) for i in range(2)]
            for ctg in range(16):
                for r in range(NCORES):
                    yc = ycand[r % 2]
                    S.dma("sp", yc[:], ybout.ap()[ctg * 128:(ctg + 1) * 128, r * TC + tok0:r * TC + tok0 + TOWN],
                          reads=[b_ybout], writes=[yc.b])
                    if r == 0:
                        TS("dve", ybT[:, ctg, :], yc[:], csel[:, 0:1], None, ALU.mult, None, [yc.b, csel.b], [ybT.b])
                    else:
                        STT(ybT[:, ctg, :], yc[:], csel[:, r:r + 1], ybT[:, ctg, :], ALU.mult, ALU.add,
                            [yc.b, csel.b, ybT.b], [ybT.b])
            S.barrier()

        with ExitStack() as ph:
            def pt(name, shape, dt=F32):
                return Tl(nc, ph, name, shape, dt)
            Wo = pt("Wo", [128, KC, 512], BF16)
            stg = [pt("stgO%d" % i, [128, 512]) for i in range(2)]
            g1bc = pt("g1bc", [128, D])
            xs_ = [pt("xsl%d" % i, [128, 512]) for i in range(2)]
            x2t = [pt("x2t%d" % i, [128, 512]) for i in range(2)]
            bc_row(g1bc[:], agout.ap()[64:96, :].rearrange("(o a) b -> o (a b)", o=1), [b_agout], [g1bc.b])
            for db in range(8):
                ds_ = slice(db * 512, (db + 1) * 512)
                load_w_bf16(lambda c: (Wo[:, c, :], Wo.b), lambda c, ds_=ds_: w_out_d[c * 128:(c + 1) * 128, ds_], KC, 512, stg)
                for tt in range(8):
                    ts_ = slice(tt * 128, (tt + 1) * 128)
                    p = ps()
                    for k in range(KC):
                        src = yaT if k < 16 else ybT
                        MM(p[:, :], src[:, k % 16, ts_], Wo[:, k, :], k == 0, k == KC - 1, [src.b, Wo.b], [p.b])
                    xs = xs_[tt % 2]
                    xo = x2t[tt % 2]
                    S.dma("sp", xs[:], x_own[tok0 + tt * 128:tok0 + (tt + 1) * 128, ds_], writes=[xs.b])
                    TT("dve", xo[:], p[:, :], g1bc[:, ds_], ALU.mult, [p.b, g1bc.b], [xo.b])
                    TT("pool", xo[:], xo[:], xs[:], ALU.add, [xo.b, xs.b], [xo.b])
                    S.dma("act", x2_d.ap()[tok0 + tt * 128:tok0 + (tt + 1) * 128, ds_], xo[:], reads=[xo.b], writes=[b_x2], keep_w=True)
            S.barrier()
        MID.close()


    if stop == "O":
        dx = dbg_out("x2", [NOB_RUN * TOWN, D])
        S.dma("sp", dx, x2_d.ap()[0:NOB_RUN * TOWN, :], reads=[b_x2], writes=[Buf("dbgx2")])
        return finish()

    NBE = TC // HALF
    if nob:
        NBE = 2 * nob
    if nblk:
        NBE = 1
    with ExitStack() as ph:
        def pt(name, shape, dt=F32, st=None):
            return Tl(nc, st or ph, name, shape, dt)
        h2T = pt("h2T", [128, KC, HALF], BF16)
        acc = pt("acc", [128, 4, D])
        Gm = pt("Gm", [128, 4, NEO])
        for tb in range(NBE):
            with ExitStack() as rs:
                h2f = pt("h2f", [128, KC, 128], F32, rs)
                rw = pt("rw", [128, KC, NE], F32, rs)
                rbb = pt("rbb", [128, NE], F32, rs)
                xt = pt("xtM", [128, D], F32, rs)
                sc = pt("sc", [128, NE], F32, rs); sel = pt("sel", [128, NE], F32, rs); selm = pt("selm_", [128, NE], F32, rs)
                m8 = pt("m8", [128, 8, 8], F32, rs); gs = pt("gs", [128, 8], F32, rs); g8 = pt("g8", [128, 8], F32, rs)
                gmask = pt("gmask", [128, 8], F32, rs); goff = pt("goff", [128, 8], F32, rs); t8 = pt("t8", [128, 8], F32, rs)
                smask = pt("smask", [128, NE], F32, rs); gsum = pt("gsum", [128, 1], F32, rs)
                S.dma("sp", rw[:], rw_d.rearrange("(k p) e -> p k e", p=128), writes=[rw.b])
                bc_row(rbb[:], rb_d, [], [rbb.b])
                for tt in range(4):
                    r0 = tb * HALF + tt * 128
                    S.dma("sp", xt[:], x2_d.ap()[r0:r0 + 128, :], reads=[b_x2], writes=[xt.b])
                    norm_T(xt, s2, sh2, modT.b,
                           [(lambda c, tt=tt: h2T[:, c, tt * 128:(tt + 1) * 128], h2T.b),
                            (lambda c: h2f[:, c, :], h2f.b)])
                    p = ps()
                    for k in range(KC):
                        MM(p[:, 0:NE], h2f[:, k, :], rw[:, k, :], k == 0, k == KC - 1, [h2f.b, rw.b], [p.b])
                    ACT(sc[:], p[:, 0:NE], AF.Sigmoid, [p.b], [sc.b])
                    TT("dve", sel[:], sc[:], rbb[:], ALU.add, [sc.b, rbb.b], [sel.b])
                    for g in range(8):
                        S.op("dve", lambda e, g=g: e.max(out=m8[:, g, :], in_=sel[:, g * 16:(g + 1) * 16]), [sel.b], [m8.b])
                    TT("dve", gs[:], m8[:, :, 0], m8[:, :, 1], ALU.add, [m8.b], [gs.b])
                    S.op("dve", lambda e: e.max(out=g8[:], in_=gs[:]), [gs.b], [g8.b])
                    TS("dve", gmask[:], gs[:], g8[:, 3:4], None, ALU.is_ge, None, [gs.b, g8.b], [gmask.b])
                    TS("dve", goff[:], gmask[:], 4.0, -4.0, ALU.mult, ALU.add, [gmask.b], [goff.b])
                    for g in range(8):
                        gsl = slice(g * 16, (g + 1) * 16)
                        TS("dve", selm[:, gsl], sel[:, gsl], gmask[:, g:g + 1], goff[:, g:g + 1], ALU.mult, ALU.add,
                           [sel.b, gmask.b, goff.b], [selm.b])
                    S.op("dve", lambda e: e.max(out=t8[:], in_=selm[:]), [selm.b], [t8.b])
                    TS("dve", smask[:], selm[:], t8[:, 7:8], None, ALU.is_ge, None, [selm.b, t8.b], [smask.b])
                    TT("dve", smask[:], smask[:], sc[:], ALU.mult, [smask.b, sc.b], [smask.b])
                    S.op("dve", lambda e: e.reduce_sum(out=gsum[:], in_=smask[:], axis=AX.X), [smask.b], [gsum.b])
                    RECIP(gsum[:], gsum[:], [gsum.b], [gsum.b])
                    TS("dve", Gm[:, tt, 0:NEO - 1], smask[:, 0:NEO - 1], gsum[:, 0:1], 2.5, ALU.mult, ALU.mult,
                       [smask.b, gsum.b], [Gm.b])
                    MEMSET("pool", Gm[:, tt, NEO - 1:NEO], 1.0, [Gm.b])
                S.barrier()
            if stop == "R":
                dg = dbg_out("Gm", [128, 4 * NEO])
                S.dma("sp", dg, Gm[:].rearrange("p a b -> p (a b)"), reads=[Gm.b], writes=[Buf("dbgg")])
                return finish()
            with ExitStack() as xs_:
                Wg = pt("Wg", [128, KC, DE], BF16, xs_); Wu_ = pt("Wu_", [128, KC, DE], BF16, xs_)
                Wd = pt("Wd", [128, 3, D], BF16, xs_)
                stg = [pt("stgE%d" % i, [128, 2048], F32, xs_) for i in range(2)]
                hb = pt("hb", [128, 3, HALF], BF16, xs_); sl = pt("sl", [128, HALF], F32, xs_)
                for e_ in range(NEO):
                    load_w_bf16(lambda c: (Wg[:, c, :], Wg.b), lambda c, e_=e_: ewg_d[e_, c * 128:(c + 1) * 128, :], KC, DE, stg)
                    load_w_bf16(lambda c: (Wu_[:, c, :], Wu_.b), lambda c, e_=e_: ewu_d[e_, c * 128:(c + 1) * 128, :], KC, DE, stg)
                    load_w_bf16(lambda c: (Wd[:, c // 2, (c % 2) * 2048:(c % 2 + 1) * 2048], Wd.b),
                                lambda c, e_=e_: ewd_d[e_, (c // 2) * 128:(c // 2 + 1) * 128, (c % 2) * 2048:(c % 2 + 1) * 2048],
                                6, 2048, stg)
                    for f in range(3):
                        fs = slice(f * 128, (f + 1) * 128)
                        pg, pu = ps(), ps()
                        for k in range(KC):
                            MM(pg[:, :], Wg[:, k, fs], h2T[:, k, :], k == 0, k == KC - 1, [Wg.b, h2T.b], [pg.b])
                        for k in range(KC):
                            MM(pu[:, :], Wu_[:, k, fs], h2T[:, k, :], k == 0, k == KC - 1, [Wu_.b, h2T.b], [pu.b])
                        ACT(sl[:], pg[:, :], AF.Silu, [pg.b], [sl.b])
                        TT("dve", hb[:, f, :], pu[:, :], sl[:], ALU.mult, [pu.b, sl.b], [hb.b])
                    for tt in range(4):
                        for db in range(8):
                            p = ps()
                            for f in range(3):
                                MM(p[:, :], hb[:, f, tt * 128:(tt + 1) * 128], Wd[:, f, db * 512:(db + 1) * 512], f == 0, f == 2,
                                   [hb.b, Wd.b], [p.b])
                            dsl = slice(db * 512, (db + 1) * 512)
                            if e_ == 0:
                                TS("dve", acc[:, tt, dsl], p[:, :], Gm[:, tt, e_:e_ + 1], None, ALU.mult, None,
                                   [p.b, Gm.b], [acc.b])
                            else:
                                STT(acc[:, tt, dsl], p[:, :], Gm[:, tt, e_:e_ + 1], acc[:, tt, dsl], ALU.mult, ALU.add,
                                    [p.b, Gm.b, acc.b], [acc.b])
                S.barrier()
            with ExitStack() as fs_:
                g2bc = pt("g2bc", [128, D], F32, fs_); fbc = pt("fbc", [128, D], F32, fs_); xt = pt("xtF", [128, D], F32, fs_)
                bc_row(g2bc[:], agout.ap()[160:192, :].rearrange("(o a) b -> o (a b)", o=1), [b_agout], [g2bc.b])
                bc_row(fbc[:], fing_d, [], [fbc.b])
                for tt in range(4):
                    r0 = tb * HALF + tt * 128
                    S.dma("sp", xt[:], x2_d.ap()[r0:r0 + 128, :], reads=[b_x2], writes=[xt.b])
                    if stop == "E":
                        S.dma("sp", dbg_out("acc%d" % tt, [128, D]), acc[:, tt, :], reads=[acc.b], writes=[Buf("dbgacc")])
                    TT("pool", acc[:, tt, :], acc[:, tt, :], g2bc[:], ALU.mult, [acc.b, g2bc.b], [acc.b])
                    TT("dve", xt[:], xt[:], acc[:, tt, :], ALU.add, [xt.b, acc.b], [xt.b])
                    ACT(junk[:], xt[:], AF.Square, [xt.b], [junk.b, ssq.b], accum_out=ssq[:])
                    TS("dve", rstd[:], ssq[:], 1.0 / D, NORM_EPS, ALU.mult, ALU.add, [ssq.b], [rstd.b])
                    ACT(rstd[:], rstd[:], AF.Sqrt, [rstd.b], [rstd.b])
                    RECIP(rstd[:], rstd[:], [rstd.b], [rstd.b])
                    STT(xt[:], xt[:], rstd[:, 0:1], fbc[:], ALU.mult, ALU.mult, [xt.b, rstd.b, fbc.b], [xt.b])
                    S.dma("sp", out_d[r0:r0 + 128, :], xt[:], reads=[xt.b], writes=[b_out], keep_w=True)
                S.barrier()
    return finish()


def _consts():
    ident = np.eye(128, dtype=np.float32)
    bones = np.zeros((128, 128), np.float32)
    bones[:64, :64] = 1.0
    bones[64:, 64:] = 1.0
    s = np.arange(128)[:, None]
    t = np.arange(128)[None, :]
    mS = (s < t).astype(np.float32)
    mI = (s <= t).astype(np.float32)
    mask4 = np.concatenate([mS, mI, mS, mI], axis=1)
    mL = (t < s).astype(np.float32)
    maskl = np.concatenate([mL, mL], axis=1)
    i64s = np.concatenate([np.eye(64, dtype=np.float32)] * 2, axis=0)
    return dict(ident=ident, bones=bones, mask4=mask4, maskl=maskl, i64s=i64s)


_NC_CACHE = {}


def make_in_maps(x, c, mod_w, mod_b, norm1_g, norm2_g, w_in, w_out,
           a_ln_g, a_ln_b, a_spatial_w, a_spatial_b,
           b_shift_mu, b_decay_up, b_decay_base, b_iclr_up, b_iclr_base, b_gate_up,
           b_kk_scale, b_ka_scale, b_bonus, b_gn_g, b_gn_b,
           router_w, router_bias, exp_w_gate, exp_w_up, exp_w_down,
           sh_w_gate, sh_w_up, sh_w_down, final_g, _names=None):
    f = lambda a: np.ascontiguousarray(np.asarray(a, dtype=np.float32))
    x = f(x)[0]
    c = f(c)[0]
    mod_w, mod_b = f(mod_w)[0], f(mod_b)[0]
    w_in, w_out = f(w_in)[0], f(w_out)[0]
    mu = f(b_shift_mu)[0]
    dup, iup, gup = f(b_decay_up)[0], f(b_iclr_up)[0], f(b_gate_up)[0]
    BO = 2 * A_W
    consts = _consts()
    want_e = _names is None or "ewg" in _names
    if want_e:
        ewg_, ewu_, ewd_ = f(exp_w_gate)[0], f(exp_w_up)[0], f(exp_w_down)[0]
        shg, shu, shd = f(sh_w_gate), f(sh_w_up), f(sh_w_down)
    rw_full, rb_full = f(router_w)[0], f(router_bias)[0]
    pad32 = lambda a: np.concatenate([a, np.zeros((a.shape[0], 128 - a.shape[1]), np.float32)], axis=1)
    def b_cols(i):
        return np.concatenate([
            w_in[:, BO + i * 256:BO + (i + 1) * 256],
            w_in[:, BO + B_W + i * 256:BO + B_W + (i + 1) * 256],
            w_in[:, BO + 2 * B_W + i * 256:BO + 2 * B_W + (i + 1) * 256],
            pad32(w_in[:, BO + 3 * B_W:BO + 3 * B_W + DLORA]),
            pad32(w_in[:, BO + 3 * B_W + DLORA:BO + 3 * B_W + 2 * DLORA]),
            w_in[:, BO + 3 * B_W + 2 * DLORA:]], axis=1)

    def mu_rows(i):
        z32 = np.zeros(32, np.float32)
        return np.concatenate([mu[i * 256:(i + 1) * 256], mu[B_W + i * 256:B_W + (i + 1) * 256],
                               mu[2 * B_W + i * 256:2 * B_W + (i + 1) * 256],
                               mu[3 * B_W:3 * B_W + DLORA], z32, mu[3 * B_W + DLORA:3 * B_W + 2 * DLORA], z32,
                               mu[3 * B_W + 2 * DLORA:]]).reshape(10, 128)

    def vec_rows(i):
        hs = slice(i * 256, (i + 1) * 256)
        return np.concatenate([f(b_decay_base)[0][hs], f(b_iclr_base)[0][hs], f(b_kk_scale)[0][hs],
                               f(b_ka_scale)[0][hs], f(b_bonus)[0].reshape(-1)[hs], f(b_gn_g)[0][hs],
                               f(b_gn_b)[0][hs], np.zeros(256, np.float32)]).reshape(16, 128)

    m = dict(
        x=x, c=c.reshape(KC, 128), mod_w=mod_w, mod_b=mod_b.reshape(1, NMOD * D),
        n1g=f(norm1_g)[0].reshape(KC, 128), n2g=f(norm2_g)[0].reshape(KC, 128), fing=f(final_g).reshape(1, D),
        w_in_a=np.ascontiguousarray(w_in[:, :BO]),
        w_in_b=np.ascontiguousarray(np.stack([b_cols(i) for i in range(NHG)])),
        mu=np.ascontiguousarray(np.stack([mu_rows(i) for i in range(NHG)])),
        w_out=w_out,
        alng=f(a_ln_g)[0].reshape(1, A_W), alnb=f(a_ln_b)[0].reshape(1, A_W),
        spw=f(a_spatial_w)[0], spb=f(a_spatial_b)[0].reshape(1, A_W),
        dup=np.ascontiguousarray(np.stack([dup[:, i * 256:(i + 1) * 256] for i in range(NHG)])),
        iup=np.ascontiguousarray(np.stack([iup[:, i * 256:(i + 1) * 256] for i in range(NHG)])),
        gup=np.ascontiguousarray(np.stack([gup[:, i * 256:(i + 1) * 256] for i in range(NHG)])),
        vecs=np.ascontiguousarray(np.stack([vec_rows(i) for i in range(NHG)])),
        rw=rw_full, rb=rb_full.reshape(1, NE), **consts)
    if want_e:
        m["ewg"] = np.concatenate([ewg_, shg], axis=0)
        m["ewu"] = np.concatenate([ewu_, shu], axis=0)
        m["ewd"] = np.concatenate([ewd_, shd], axis=0)
    if _names is not None:
        m = {k: v for k, v in m.items() if k in _names}
    in_maps = []
    for i in range(NCORES):
        mi = dict(m)
        if _names is None or "x_own" in _names:
            mi["x_own"] = np.ascontiguousarray(x[i * TC:(i + 1) * TC])
            cs = np.zeros((128, NCORES), np.float32)
            cs[:, i] = 1.0
            mi["csel"] = cs
        in_maps.append(mi)
    return in_maps


def kernel(**inputs):
    in_maps = make_in_maps(**inputs)
    if "nc" not in _NC_CACHE:
        _NC_CACHE["nc"] = build_nc()
    res = run_bass_kernel_spmd(_NC_CACHE["nc"], in_maps, core_ids=list(range(NCORES)))
    out = np.concatenate([np.asarray(r["out"], dtype=np.float32) for r in res.results], axis=0)
    return out.reshape(1, T, D)
```
